# Optimizing a Trainium2 kernel written in Bass

```python
import jax, jax.numpy as jnp
from jax import lax
import numpy as np

D_MODEL = 1024
BATCH = 8
SEQ = 2048
DEPTH = 1

EPS = 1e-6
A_HEADS = 16
A_QK_DIM = 64
A_V_DIM = 64
A_Q_RANK = 256
A_KV_RANK = 256
IDX_HEADS = 8
IDX_DIM = 64
TOPK_MAX = 256
Q_BLOCK = 128
A_WIDTH = A_HEADS * A_V_DIM
B_HEADS = 8
B_K_DIM = 128
B_V_DIM = 128
B_QK = B_HEADS * B_K_DIM
B_VW = B_HEADS * B_V_DIM
CONV_WIDTH = 4
CHUNK = 64
D_FF = ((8 * D_MODEL // 3 + 255) // 256) * 256
IN_SIZES = (A_Q_RANK, A_KV_RANK, IDX_DIM, IDX_HEADS,
            B_QK, B_QK, B_VW, B_HEADS, B_HEADS, B_VW,
            D_MODEL, D_MODEL)
IN_WIDTH = (A_Q_RANK + A_KV_RANK + IDX_DIM + IDX_HEADS
            + 2 * B_QK + 2 * B_VW + 2 * B_HEADS + 2 * D_MODEL)

kernel_name = "hybrid_dsa_gdn_gated_merge"


def rmsnorm(x, g):
    xf = x.astype(jnp.float32)
    y = xf * lax.rsqrt(jnp.mean(xf * xf, axis=-1, keepdims=True) + EPS)
    return (y * g.astype(jnp.float32)).astype(x.dtype)


def layernorm(x, g, b):
    xf = x.astype(jnp.float32)
    mu = jnp.mean(xf, axis=-1, keepdims=True)
    xc = xf - mu
    y = xc * lax.rsqrt(jnp.mean(xc * xc, axis=-1, keepdims=True) + EPS)
    return (y * g.astype(jnp.float32) + b.astype(jnp.float32)).astype(x.dtype)


def l2norm(x):
    xf = x.astype(jnp.float32)
    return xf * lax.rsqrt(jnp.sum(xf * xf, axis=-1, keepdims=True) + EPS)


def dsa_attention(c_q, c_kv, k_idx_raw, w_idx_raw, cq_g, ckv_g, w_uq, w_uk, w_uv,
                  w_iq, kln_g, kln_b):
    B, L, _ = c_q.shape
    topk = min(TOPK_MAX, L // 4)
    nb = L // Q_BLOCK
    cq = rmsnorm(c_q, cq_g)
    ckv = rmsnorm(c_kv, ckv_g)
    q = (cq @ w_uq).reshape(B, L, A_HEADS, A_QK_DIM)
    q_lat = jnp.einsum('blhd,hdr->blhr', q, w_uk)
    q_idx = (cq @ w_iq).reshape(B, L, IDX_HEADS, IDX_DIM)
    k_idx = layernorm(k_idx_raw, kln_g, kln_b).astype(jnp.float32)
    w_idx = w_idx_raw * (IDX_HEADS ** -0.5 * IDX_DIM ** -0.5)
    key_pos = jnp.arange(L)

    def to_blocks(t):
        return t.reshape(B, nb, Q_BLOCK, *t.shape[2:]).swapaxes(0, 1)

    def block(args):
        i, ql, qi, wi = args
        q_pos = i * Q_BLOCK + jnp.arange(Q_BLOCK)
        causal = key_pos[None, :] <= q_pos[:, None]
        logits = jnp.einsum('bqhd,bsd->bqhs', qi.astype(jnp.float32), k_idx)
        score = jnp.einsum('bqh,bqhs->bqs', wi.astype(jnp.float32), jax.nn.relu(logits))
        score = jnp.where(causal[None], score, -jnp.inf)
        _, sel = lax.top_k(score, topk)
        valid = sel <= q_pos[None, :, None]
        kv_sel = jax.vmap(lambda kv, ix: kv[ix])(ckv, sel)
        s = jnp.einsum('bqhr,bqkr->bqhk', ql, kv_sel).astype(jnp.float32) * (A_QK_DIM ** -0.5)
        s = jnp.where(valid[:, :, None, :], s, -jnp.inf)
        p = jax.nn.softmax(s, axis=-1).astype(ckv.dtype)
        return jnp.einsum('bqhk,bqkr->bqhr', p, kv_sel)

    o_lat = lax.map(block, (jnp.arange(nb), to_blocks(q_lat), to_blocks(q_idx), to_blocks(w_idx)))
    o_lat = o_lat.swapaxes(0, 1).reshape(B, L, A_HEADS, A_KV_RANK)
    o = jnp.einsum('blhr,hrd->blhd', o_lat, w_uv)
    return o.reshape(B, L, A_WIDTH)


def short_conv(x, w):
    C = x.shape[-1]
    return lax.conv_general_dilated(
        x, w[:, None, :].astype(x.dtype), window_strides=(1,),
        padding=[(CONV_WIDTH - 1, 0)], dimension_numbers=('NWC', 'WIO', 'NWC'),
        feature_group_count=C)


def gated_delta_rule(q, k, v, g, beta):
    B, L, H, Dk = q.shape
    Dv = v.shape[-1]
    N = L // CHUNK
    def ch4(t):
        return t.reshape(B, N, CHUNK, H, t.shape[-1]).transpose(1, 0, 3, 2, 4)
    def ch3(t):
        return t.reshape(B, N, CHUNK, H).transpose(1, 0, 3, 2)
    qc = ch4(q * (Dk ** -0.5))
    kc, vc = ch4(k), ch4(v)
    bc = ch3(beta)
    G = jnp.cumsum(ch3(g), axis=-1)
    tri = jnp.tril(jnp.ones((CHUNK, CHUNK), bool))
    strict = jnp.tril(jnp.ones((CHUNK, CHUNK), bool), -1)
    decay = jnp.exp(jnp.where(tri, G[..., :, None] - G[..., None, :], -jnp.inf))
    kk = jnp.einsum('nbhcd,nbhsd->nbhcs', kc, kc)
    A = jnp.where(strict, bc[..., None] * kk * decay, 0.0) + jnp.eye(CHUNK, dtype=jnp.float32)
    rhs = jnp.concatenate([vc * bc[..., None], kc * (bc * jnp.exp(G))[..., None]], axis=-1)
    sol = lax.linalg.triangular_solve(A, rhs, left_side=True, lower=True, unit_diagonal=True)
    u_base, w_dec = sol[..., :Dv], sol[..., Dv:]
    qk = jnp.einsum('nbhcd,nbhsd->nbhcs', qc, kc) * decay
    q_dec = qc * jnp.exp(G)[..., None]
    k_tail = kc * jnp.exp(G[..., -1:] - G)[..., None]
    a_tail = jnp.exp(G[..., -1])

    def step(S, xs):
        u_b, w_d, qk_i, qd, kt, at = xs
        u = u_b - jnp.einsum('bhcd,bhdv->bhcv', w_d, S)
        o = jnp.einsum('bhcd,bhdv->bhcv', qd, S) + jnp.einsum('bhcs,bhsv->bhcv', qk_i, u)
        S = S * at[..., None, None] + jnp.einsum('bhcd,bhcv->bhdv', kt, u)
        return S, o

    S0 = jnp.zeros((B, H, Dk, Dv), jnp.float32)
    _, o = lax.scan(step, S0, (u_base, w_dec, qk, q_dec, k_tail, a_tail))
    return o.transpose(1, 0, 3, 2, 4).reshape(B, L, H, Dv)


def gated_deltanet(q, k, v, b, a, z, conv_w, a_log, dt_bias, onorm_g):
    B, L, _ = q.shape
    dt = q.dtype
    qkv = jax.nn.silu(short_conv(jnp.concatenate([q, k, v], axis=-1), conv_w))
    qs, ks, vs = jnp.split(qkv, [B_QK, 2 * B_QK], axis=-1)
    qs = l2norm(qs.reshape(B, L, B_HEADS, B_K_DIM))
    ks = l2norm(ks.reshape(B, L, B_HEADS, B_K_DIM))
    vs = vs.reshape(B, L, B_HEADS, B_V_DIM).astype(jnp.float32)
    beta = jax.nn.sigmoid(b.astype(jnp.float32))
    g = -jnp.exp(a_log.astype(jnp.float32)) * jax.nn.softplus(a.astype(jnp.float32) + dt_bias.astype(jnp.float32))
    o = gated_delta_rule(qs, ks, vs, g, beta)
    o = rmsnorm(o, onorm_g) * jax.nn.silu(z.reshape(B, L, B_HEADS, B_V_DIM).astype(jnp.float32))
    return o.reshape(B, L, B_VW).astype(dt)


def _w(k, shape, fan_in):
    return jax.random.normal(k, shape, jnp.float32) * (fan_in ** -0.5)


def _gain(k, shape):
    return 1.0 + 0.02 * jax.random.normal(k, shape, jnp.float32)


def setup_inputs(seed: int = 0) -> dict:
    key = jax.random.key(seed)
    ks = jax.random.split(key, 24)
    Dp = DEPTH
    dt0 = jnp.exp(jax.random.uniform(ks[14], (Dp, B_HEADS), jnp.float32, np.log(1e-3), np.log(1e-1)))
    return {
        "x": jax.random.normal(ks[0], (BATCH, SEQ, D_MODEL), jnp.float32),
        "mix_norm_g": _gain(ks[1], (Dp, D_MODEL)),
        "w_in": _w(ks[2], (Dp, D_MODEL, IN_WIDTH), D_MODEL),
        "cq_norm_g": _gain(ks[3], (Dp, A_Q_RANK)),
        "ckv_norm_g": _gain(ks[4], (Dp, A_KV_RANK)),
        "w_uq": _w(ks[5], (Dp, A_Q_RANK, A_HEADS * A_QK_DIM), A_Q_RANK),
        "w_uk": _w(ks[6], (Dp, A_HEADS, A_QK_DIM, A_KV_RANK), A_KV_RANK),
        "w_uv": _w(ks[7], (Dp, A_HEADS, A_KV_RANK, A_V_DIM), A_KV_RANK),
        "w_iq": _w(ks[8], (Dp, A_Q_RANK, IDX_HEADS * IDX_DIM), A_Q_RANK),
        "kidx_ln_g": _gain(ks[9], (Dp, IDX_DIM)),
        "kidx_ln_b": 0.02 * jax.random.normal(ks[10], (Dp, IDX_DIM), jnp.float32),
        "w_branch_a": _w(ks[11], (Dp, A_WIDTH, D_MODEL), A_WIDTH),
        "conv_w": _w(ks[12], (Dp, CONV_WIDTH, 2 * B_QK + B_VW), CONV_WIDTH),
        "a_log": jnp.log(jax.random.uniform(ks[13], (Dp, B_HEADS), jnp.float32, 1.0, 16.0)),
        "dt_bias": dt0 + jnp.log(-jnp.expm1(-dt0)),
        "onorm_g": _gain(ks[15], (Dp, B_V_DIM)),
        "w_branch_b": _w(ks[16], (Dp, B_VW, D_MODEL), B_VW),
        "w_out": _w(ks[17], (Dp, D_MODEL, D_MODEL), D_MODEL),
        "ffn_norm_g": _gain(ks[18], (Dp, D_MODEL)),
        "w_gate": _w(ks[19], (Dp, D_MODEL, D_FF), D_MODEL),
        "w_up": _w(ks[20], (Dp, D_MODEL, D_FF), D_MODEL),
        "w_down": _w(ks[21], (Dp, D_FF, D_MODEL), D_FF),
        "final_norm_g": _gain(ks[22], (D_MODEL,)),
    }


def reference(x, mix_norm_g, w_in, cq_norm_g, ckv_norm_g, w_uq, w_uk, w_uv, w_iq,
              kidx_ln_g, kidx_ln_b, w_branch_a, conv_w, a_log, dt_bias, onorm_g,
              w_branch_b, w_out, ffn_norm_g, w_gate, w_up, w_down, final_norm_g):
    splits = [int(s) for s in np.cumsum(IN_SIZES)[:-1]]
    for l in range(DEPTH):
        h = rmsnorm(x, mix_norm_g[l])
        proj = h @ w_in[l]
        (c_q, c_kv, k_idx, w_idx, q_b, k_b, v_b, beta_b, a_b, z_b,
         gate_a, gate_b) = jnp.split(proj, splits, axis=-1)
        o_a = dsa_attention(c_q, c_kv, k_idx, w_idx, cq_norm_g[l], ckv_norm_g[l],
                            w_uq[l], w_uk[l], w_uv[l], w_iq[l], kidx_ln_g[l], kidx_ln_b[l])
        o_b = gated_deltanet(q_b, k_b, v_b, beta_b, a_b, z_b, conv_w[l], a_log[l],
                             dt_bias[l], onorm_g[l])
        merged = (jax.nn.sigmoid(gate_a) * (o_a @ w_branch_a[l])
                  + jax.nn.sigmoid(gate_b) * (o_b @ w_branch_b[l]))
        x = x + merged @ w_out[l]
        h = rmsnorm(x, ffn_norm_g[l])
        x = x + (jax.nn.silu(h @ w_gate[l]) * (h @ w_up[l])) @ w_down[l]
    return rmsnorm(x, final_norm_g)
```

```python
from contextlib import ExitStack

import numpy as np
import concourse.bass as bass
import concourse.mybir as mybir
from concourse.bass_utils import run_bass_kernel_spmd

F32 = mybir.dt.float32
BF16 = mybir.dt.bfloat16
AF = mybir.ActivationFunctionType
ALU = mybir.AluOpType
AX_ = mybir.AxisListType

L = 2048
D = 1024
NT = 16
EPS = 1e-6
DFF = 2816
NFC = DFF // 128
INW = 6744
C_Q, C_KV, C_KI, C_WI = 0, 256, 512, 576
C_QB, C_KB, C_VB = 584, 1608, 2632
C_BETA, C_A = 3656, 3664
C_Z, C_GA, C_GB = 3672, 4696, 5720
IDX_SCALE = float(8 ** -0.5 * 64 ** -0.5)
NEG = -30000.0
NBIS = 22

DEBUG = False
DEBUG_FLUSH = False
STOP_AFTER_FLUSH = 0


class _Stop(Exception):
    pass


class T:
    __slots__ = ("w", "r", "excl")

    def __init__(self, excl=False):
        self.w = None
        self.r = {}
        self.excl = excl


def Ts(n, excl=False):
    return [T(excl) for _ in range(n)]


class FW:
    ENGS = ("tensor", "vector", "scalar", "gpsimd", "sync")
    NDMA = 8

    def __init__(self, nc, stack):
        self.nc = nc
        self.ops = {e: [] for e in self.ENGS}
        self.cnt = {e: 0 for e in self.ENGS}
        self.waited = {e: {} for e in self.ENGS}
        self.dma_i = 0
        self.final = {}
        keys = list(self.ENGS) + ["dma%d" % i for i in range(self.NDMA)]
        self.sems = {k: stack.enter_context(nc.semaphore("s_" + k)) for k in keys}
        self.nops = 0
        self.nflush = 0
        self.rank_base = {e: 0 for e in self.ENGS}
        self.flushed = {e: 0 for e in self.ENGS}

    def _need(self, eng, dep, waits):
        if dep is None:
            return
        k, v = dep
        if self.waited[eng].get(k, 0) >= v:
            return
        if k == eng and eng == "tensor":
            return
        waits[k] = max(waits.get(k, 0), v)

    def _deps(self, eng, reads, writes):
        waits = {}
        for t in reads:
            self._need(eng, t.w, waits)
            if t.excl:
                for k, v in t.r.items():
                    if k != eng:
                        self._need(eng, (k, v), waits)
        for t in writes:
            self._need(eng, t.w, waits)
            for k, v in t.r.items():
                self._need(eng, (k, v), waits)
        for k, v in waits.items():
            self.waited[eng][k] = v
        return waits

    def _mark(self, key, val, reads, writes):
        for t in reads:
            t.r[key] = max(t.r.get(key, 0), val)
        for t in writes:
            t.w = (key, val)
            t.r = {}
        self.final[key] = max(self.final.get(key, 0), val)

    def op(self, eng, name, kw, reads=(), writes=()):
        reads = tuple(reads)
        writes = tuple(writes)
        waits = self._deps(eng, reads, writes)
        self.cnt[eng] += 1
        self.ops[eng].append((tuple(waits.items()), name, kw, eng, self.cnt[eng]))
        self._mark(eng, self.cnt[eng], reads, writes)
        self.nops += 1

    def dma(self, out, in_, reads=(), writes=()):
        reads = tuple(reads)
        writes = tuple(writes)
        i = self.dma_i
        self.dma_i += 1
        key = "dma%d" % (i % self.NDMA)
        val = 16 * (i // self.NDMA + 1)
        waits = self._deps("sync", reads, writes)
        if val > 16 and self.waited["sync"].get(key, 0) < val - 16:
            waits[key] = val - 16
            self.waited["sync"][key] = val - 16
        self.cnt["sync"] += 1
        self.ops["sync"].append((tuple(waits.items()), "dma_start", dict(out=out, in_=in_), key, self.cnt["sync"]))
        self._mark(key, val, reads, writes)
        self.nops += 1

    def flush(self):
        nc = self.nc
        sems = self.sems
        ops = self.ops
        ms = {e: set() for e in self.ENGS}
        for e in self.ENGS:
            for waits, _n, _kw, _key, _idx in ops[e]:
                for k, v in waits:
                    if k in ms:
                        ms[k].add(v)
        for e in self.ENGS:
            if e != "sync" and self.cnt[e] > self.flushed[e]:
                ms[e].add(self.cnt[e])
        rank = {}
        for e in self.ENGS:
            for i, v in enumerate(sorted(ms[e])):
                rank[(e, v)] = self.rank_base[e] + i + 1
        final_waits = []
        for e in self.ENGS:
            if e != "sync" and self.cnt[e] > self.flushed[e]:
                final_waits.append((sems[e], rank[(e, self.cnt[e])]))
        for k, v in self.final.items():
            if k.startswith("dma"):
                final_waits.append((sems[k], v))

        def wval(k, v):
            return v if k.startswith("dma") else rank[(k, v)]

        with nc.Block() as block:
            def body(ename):
                def f(e):
                    for waits, name, kw, key, idx in ops[ename]:
                        for k, v in waits:
                            e.wait_ge(sems[k], wval(k, v))
                        ins = getattr(e, name)(**kw)
                        if key.startswith("dma"):
                            ins.then_inc(sems[key], 16)
                        elif (ename, idx) in rank:
                            ins.then_inc(sems[ename], 1)
                    if ename == "sync":
                        for s, v in final_waits:
                            e.wait_ge(s, v)
                return f
            block.tensor(body("tensor"))
            block.vector(body("vector"))
            block.scalar(body("scalar"))
            block.gpsimd(body("gpsimd"))
            block.sync(body("sync"))
        for e in self.ENGS:
            self.rank_base[e] += len(ms[e])
            self.flushed[e] = self.cnt[e]
        self.ops = {e: [] for e in self.ENGS}
        done = dict(self.final)
        for e in self.ENGS:
            done[e] = self.cnt[e]
        for e in self.ENGS:
            self.waited[e] = dict(done)
        self.nflush += 1
        if STOP_AFTER_FLUSH and self.nflush >= STOP_AFTER_FLUSH:
            raise _Stop()

    def clear_sems(self):
        nc = self.nc
        sems = self.sems
        with nc.Block() as block:
            def f(e):
                for s in sems.values():
                    e.sem_clear(s)
            block.sync(f)


def build_nc():
    nc = bass.Bass("TRN2", target_bir_lowering=False)

    def din(name, shape):
        return nc.dram_tensor(name, list(shape), F32, kind="ExternalInput").ap()

    x_d = din("x", [L, D])
    w_in = din("w_in", [D, INW])
    w_uq = din("w_uq", [256, 1024])
    w_uk = din("w_uk", [16, 64, 256])
    w_uv = din("w_uv", [16, 256, 64])
    w_iq = din("w_iq", [256, 512])
    w_ba = din("w_branch_a", [D, D])
    w_bb = din("w_branch_b", [D, D])
    w_out = din("w_out", [D, D])
    w_gate = din("w_gate", [D, DFF])
    w_up = din("w_up", [D, DFF])
    w_down = din("w_down", [DFF, D])
    mixg_d = din("mixg_col", [128, 8])
    ffng_d = din("ffng_col", [128, 8])
    fing_d = din("fing_row", [128, D])
    cqg_d = din("cqg_row", [128, 256])
    ckvg_d = din("ckvg_row", [128, 256])
    klng_d = din("klng_row", [128, 64])
    klnb_d = din("klnb_row", [128, 64])
    conv_d = din("conv_col", [128, 24, 4])
    alog_d = din("alog_row", [128, 8])
    dtb_d = din("dtb_row", [128, 8])
    ong_d = din("onormg_col", [128, 1])
    ident_d = din("ident", [128, 128])
    uincl_d = din("uincl", [128, 128])
    lstr_d = din("lstrict", [128, 128])
    out_d = nc.dram_tensor("out", [L, D], F32, kind="ExternalOutput").ap()
    skind = "ExternalOutput" if DEBUG else "Internal"
    x2_d = nc.dram_tensor("x2_scratch", [L, D], F32, kind=skind).ap()
    oa_d = nc.dram_tensor("oa_scratch", [8, 128, L], BF16, kind=skind).ap()
    ob_d = nc.dram_tensor("ob_scratch", [8, 128, L], BF16, kind=skind).ap()
    oa_v = oa_d.rearrange("h p t -> p h t")
    ob_v = ob_d.rearrange("h p t -> p h t")
    t_oad, t_obd, t_x2d = T(), T(), T()

    w_in_v = w_in.rearrange("(kc p) n -> p kc n", p=128)

    try:
      with ExitStack() as top:
        fw = FW(nc, top)
        fw.clear_sems()

        def sb(st, name, shape, dt):
            return st.enter_context(nc.sbuf_tensor(name, list(shape), dt))

        def pst(st, name, shape, dt):
            return st.enter_context(nc.psum_tensor(name, list(shape), dt))

        def V(name, r, w, **kw):
            fw.op("vector", name, kw, r, w)

        def A(name, r, w, **kw):
            fw.op("scalar", name, kw, r, w)

        def G(name, r, w, **kw):
            fw.op("gpsimd", name, kw, r, w)

        def P(name, r, w, **kw):
            fw.op("tensor", name, kw, r, w)

        def mm(out, lhsT, rhs, start, stop, r, w):
            fw.op("tensor", "matmul", dict(out=out, lhsT=lhsT, rhs=rhs, start=start, stop=stop), r, w)

        def rstd(t, src_, tmp_, dst_, scale, bias):
            A("activation", [t], [t], out=tmp_, in_=src_, func=AF.Ln, scale=scale, bias=bias)
            A("activation", [t], [t], out=dst_, in_=tmp_, func=AF.Exp, scale=-0.5)

        ident_f = sb(top, "ident_f", [128, 128], F32)
        ident_b = sb(top, "ident_b", [128, 128], BF16)
        ones_b = sb(top, "ones_b", [128, 128], BF16)
        ones_f = sb(top, "ones_f", [128, 128], F32)
        uincl_f = sb(top, "uincl_f", [128, 128], F32)
        lstr_f = sb(top, "lstr_f", [128, 128], F32)
        stg = [sb(top, "stg%d" % i, [128, 1024], F32) for i in range(3)]
        t_stg = Ts(3)
        stg_i = [0]
        t_const = T()
        mix = ExitStack()
        hT = sb(mix, "hT", [128, 8, L], BF16)
        t_hT = Ts(NT)
        beta = sb(mix, "beta", [128, NT, 8], F32)
        nbeta = sb(mix, "nbeta", [128, NT, 8], F32)
        gdec = sb(mix, "gdec", [128, NT, 8], F32)
        Gc = sb(mix, "Gc", [128, NT, 8], F32)
        bg = sb(mix, "bg", [128, NT, 8], F32)
        eTail = sb(mix, "eTail", [128, NT, 8], F32)
        aTail = sb(mix, "aTail", [128, NT, 8], F32)
        t_dn = T()
        att = ExitStack()
        cqT = sb(att, "cqT", [128, 2, L], BF16)
        ckvT = sb(att, "ckvT", [128, 2, L], BF16)
        ckv1 = sb(att, "ckv1", [128, NT, 256], BF16)
        kiT = sb(att, "kiT", [64, L], BF16)
        absw = sb(att, "absw", [128, NT, 8], F32)
        sgnw = sb(att, "sgnw", [128, NT, 8], F32)
        t_cqT, t_ckvT, t_ckv1, t_kiT = Ts(NT), Ts(NT), Ts(NT), Ts(NT)
        t_w = T()

        fw.dma(ident_f[:], ident_d, writes=[t_const])
        fw.dma(uincl_f[:], uincl_d, writes=[t_const])
        fw.dma(lstr_f[:], lstr_d, writes=[t_const])
        V("tensor_copy", [t_const], [t_const], out=ident_b[:], in_=ident_f[:])
        V("memset", [], [t_const], ap=ones_b[:], constant=1.0)
        V("memset", [], [t_const], ap=ones_f[:], constant=1.0)

        cast_engs = ["gpsimd"]

        def cast(dst, src_, r, w):
            e = cast_engs[stg_i[0] % len(cast_engs)]
            fw.op(e, "copy" if e == "scalar" else "tensor_copy", dict(out=dst, in_=src_), r, w)

        def wload(dst, src, t_dst, width):
            i = stg_i[0] % 3
            stg_i[0] += 1
            fw.dma(stg[i][:, 0:width], src, writes=[t_stg[i]])
            cast(dst, stg[i][:, 0:width], [t_stg[i]], [t_dst])

        def wload3(dst, src, t_dst, a, b):
            i = stg_i[0] % 3
            stg_i[0] += 1
            sv = stg[i][:, 0:a * b].rearrange("p (a b) -> p a b", a=a)
            fw.dma(sv, src, writes=[t_stg[i]])
            cast(dst, sv, [t_stg[i]], [t_dst])

        def rmsnorm_T(st_name, xt, t_xt, gB, dstT, t_dst, col0, res):
            junk, t_junk, ss, t_ss, xs, t_xs, pT, t_pT = res
            A("activation", [t_xt], [t_junk, t_ss], out=junk[:], in_=xt, func=AF.Square, accum_out=ss[:, 0:1])
            rstd(t_ss, ss[:, 0:1], ss[:, 1:2], ss[:, 3:4], 1.0 / D, EPS)
            V("tensor_scalar", [t_xt, t_ss], [t_xs], out=xs[:], in0=xt, scalar1=ss[:, 3:4], scalar2=None,
              op0=ALU.mult)
            for kc in range(8):
                P("transpose", [t_xs, t_const], [t_pT], out=pT[:, kc, :], in_=xs[:, kc * 128:(kc + 1) * 128],
                  identity=ident_b[:])
            V("tensor_tensor", [t_pT, t_const], [t_dst], out=dstT[:, :, col0:col0 + 128], in0=pT[:], in1=gB[:],
              op=ALU.mult)

        with ExitStack() as ph:
            gB = sb(ph, "gB", [128, 8, 128], F32)
            mixg = sb(ph, "mixg", [128, 8], F32)
            xin = [sb(ph, "xin%d" % i, [128, D], F32) for i in range(2)]
            t_xin = Ts(2)
            junk = sb(ph, "junk", [128, D], F32)
            ss = sb(ph, "ss", [128, 8], F32)
            xs = sb(ph, "xs", [128, D], BF16)
            pT = pst(ph, "pT", [128, 8, 128], BF16)
            res = (junk, T(), ss, T(), xs, T(), pT, T(True))
            wA = sb(ph, "wA", [128, 8, 584], BF16)
            wBA = sb(ph, "wBA", [128, 8, 16], BF16)
            t_wA = T()
            cqg = sb(ph, "cqg", [128, 256], F32)
            ckvg = sb(ph, "ckvg", [128, 256], F32)
            klng = sb(ph, "klng", [128, 64], F32)
            klnb = sb(ph, "klnb", [128, 64], F32)
            alog = sb(ph, "alog", [128, 8], F32)
            dtb = sb(ph, "dtb", [128, 8], F32)
            nalog = sb(ph, "nalog", [128, 8], F32)
            t_sm = T()
            psA = pst(ph, "psA", [128, 512], F32)
            psB = pst(ph, "psB", [128, 512], F32)
            psT2 = pst(ph, "psT2", [128, 8, 128], BF16)
            t_psA, t_psB, t_psT2 = T(True), T(True), T(True)
            st8 = sb(ph, "st8", [128, 16], F32)
            t_st8 = T()
            nrm = sb(ph, "nrm", [128, 512], BF16)
            t_nrm = T()
            kn = sb(ph, "kn", [128, 64], F32)
            knb = sb(ph, "knb", [128, 64], BF16)
            bst = sb(ph, "bst", [128, 8], F32)
            t_kn = T()
            sp = sb(ph, "sp", [128, 16], F32)
            t_sp = T()

            fw.dma(mixg[:], mixg_d, writes=[t_sm])
            for a_, b_ in ((cqg, cqg_d), (ckvg, ckvg_d), (klng, klng_d), (klnb, klnb_d), (alog, alog_d), (dtb, dtb_d)):
                fw.dma(a_[:], b_, writes=[t_sm])
            for kc in range(8):
                V("tensor_scalar", [t_const, t_sm], [t_const], out=gB[:, kc, :], in0=ones_f[:],
                  scalar1=mixg[:, kc:kc + 1], scalar2=None, op0=ALU.mult)
            A("activation", [t_sm], [t_sm], out=nalog[:], in_=alog[:], func=AF.Exp)
            V("tensor_scalar", [t_sm], [t_sm], out=nalog[:], in0=nalog[:], scalar1=-1.0, scalar2=None, op0=ALU.mult)
            for kc in range(8):
                wload(wA[:, kc, :], w_in_v[:, kc, 0:584], t_wA, 584)
                wload(wBA[:, kc, :], w_in_v[:, kc, C_BETA:C_BETA + 16], t_wA, 16)
            V("memset", [], [t_ckv1[0]], ap=absw[:], constant=0.0)

            for tt in range(NT):
                sl = slice(tt * 128, (tt + 1) * 128)
                xt = xin[tt % 2]
                fw.dma(xt[:], x_d[sl, :], writes=[t_xin[tt % 2]])
                rmsnorm_T("p1", xt[:], t_xin[tt % 2], gB, hT, t_hT[tt], tt * 128, res)
                for kc in range(8):
                    mm(psA[:], hT[:, kc, sl], wA[:, kc, 0:512], kc == 0, kc == 7, [t_hT[tt], t_wA], [t_psA])
                for kc in range(8):
                    mm(psB[:, 0:72], hT[:, kc, sl], wA[:, kc, 512:584], kc == 0, kc == 7, [t_hT[tt], t_wA], [t_psB])
                for kc in range(8):
                    mm(psB[:, 96:112], hT[:, kc, sl], wBA[:, kc, :], kc == 0, kc == 7, [t_hT[tt], t_wA], [t_psB])
                for j, gg in ((0, cqg), (1, ckvg)):
                    cs = slice(j * 256, (j + 1) * 256)
                    A("activation", [t_psA], [res[1], t_st8], out=junk[:, 0:256], in_=psA[:, cs], func=AF.Square,
                      accum_out=st8[:, 4 * j:4 * j + 1])
                    rstd(t_st8, st8[:, 4 * j:4 * j + 1], st8[:, 4 * j + 1:4 * j + 2], st8[:, 4 * j + 3:4 * j + 4],
                         1.0 / 256, EPS)
                    V("scalar_tensor_tensor", [t_psA, t_st8, t_sm], [t_nrm], out=nrm[:, cs], in0=psA[:, cs],
                      scalar=st8[:, 4 * j + 3:4 * j + 4], in1=gg[:], op0=ALU.mult, op1=ALU.mult)
                G("tensor_copy", [t_nrm], [t_ckv1[tt]], out=ckv1[:, tt, :], in_=nrm[:, 256:512])
                for j in range(4):
                    P("transpose", [t_nrm, t_const], [t_psT2], out=psT2[:, j, :], in_=nrm[:, j * 128:(j + 1) * 128],
                      identity=ident_b[:])
                A("copy", [t_psT2], [t_cqT[tt]], out=cqT[:, :, sl], in_=psT2[:, 0:2, :])
                A("copy", [t_psT2], [t_ckvT[tt]], out=ckvT[:, :, sl], in_=psT2[:, 2:4, :])
                V("bn_stats", [t_psB], [t_st8], out=st8[:, 8:14], in_=psB[:, 0:64])
                V("bn_aggr", [t_st8], [t_st8], out=st8[:, 14:16], in_=st8[:, 8:14])
                rstd(t_st8, st8[:, 15:16], st8[:, 9:10], st8[:, 10:11], 1.0, EPS)
                V("tensor_scalar", [t_psB, t_st8], [t_kn], out=kn[:], in0=psB[:, 0:64], scalar1=st8[:, 14:15],
                  scalar2=st8[:, 10:11], op0=ALU.subtract, op1=ALU.mult)
                V("tensor_tensor", [t_kn, t_sm], [t_kn], out=kn[:], in0=kn[:], in1=klng[:], op=ALU.mult)
                V("tensor_tensor", [t_kn, t_sm], [t_kn], out=knb[:], in0=kn[:], in1=klnb[:], op=ALU.add)
                P("transpose", [t_kn, t_const], [t_psT2], out=psT2[0:64, 4, :], in_=knb[:], identity=ident_b[:])
                A("copy", [t_psT2], [t_kiT[tt]], out=kiT[:, sl], in_=psT2[0:64, 4, :])
                V("tensor_scalar", [t_psB], [t_w], out=sgnw[:, tt, :], in0=psB[:, 64:72], scalar1=0.0, scalar2=2.0,
                  op0=ALU.is_ge, op1=ALU.mult)
                V("tensor_scalar", [t_w], [t_w], out=sgnw[:, tt, :], in0=sgnw[:, tt, :], scalar1=-1.0, scalar2=None,
                  op0=ALU.add)
                V("scalar_tensor_tensor", [t_psB, t_w], [t_w], out=absw[:, tt, :], in0=psB[:, 64:72], scalar=IDX_SCALE,
                  in1=sgnw[:, tt, :], op0=ALU.mult, op1=ALU.mult)
                A("activation", [t_psB], [t_sp], out=sp[:, 0:8], in_=psB[:, 96:104], func=AF.Exp, scale=-1.0)
                V("tensor_scalar", [t_sp], [t_sp], out=sp[:, 0:8], in0=sp[:, 0:8], scalar1=1.0, scalar2=None,
                  op0=ALU.add)
                V("reciprocal", [t_sp], [t_dn], out=beta[:, tt, :], in_=sp[:, 0:8])
                V("tensor_tensor", [t_psB, t_sm], [t_sp], out=sp[:, 8:16], in0=psB[:, 104:112], in1=dtb[:], op=ALU.add)
                A("activation", [t_sp], [t_sp], out=sp[:, 8:16], in_=sp[:, 8:16], func=AF.Exp)
                A("activation", [t_sp], [t_sp], out=sp[:, 8:16], in_=sp[:, 8:16], func=AF.Ln, bias=1.0)
                V("tensor_tensor", [t_sp, t_sm], [t_dn], out=gdec[:, tt, :], in0=sp[:, 8:16], in1=nalog[:], op=ALU.mult)

            gflat = gdec[:].rearrange("p a b -> p (a b)")
            Gflat = Gc[:].rearrange("p a b -> p (a b)")
            mm(psB[:, 0:128], uincl_f[:], gflat, True, True, [t_dn, t_const], [t_psB])
            V("tensor_copy", [t_psB], [t_dn], out=Gflat, in_=psB[:, 0:128])
            mm(psB[:, 0:128], ones_f[:], gflat, True, True, [t_dn, t_const], [t_psB])
            A("activation", [t_psB], [t_dn], out=aTail[:].rearrange("p a b -> p (a b)"), in_=psB[:, 0:128], func=AF.Exp)
            V("tensor_tensor", [t_psB, t_dn], [t_dn], out=eTail[:].rearrange("p a b -> p (a b)"), in0=psB[:, 0:128],
              in1=Gflat, op=ALU.subtract)
            A("activation", [t_dn], [t_dn], out=eTail[:].rearrange("p a b -> p (a b)"),
              in_=eTail[:].rearrange("p a b -> p (a b)"), func=AF.Exp)
            A("activation", [t_dn], [t_dn], out=bg[:].rearrange("p a b -> p (a b)"), in_=Gflat, func=AF.Exp)
            V("tensor_tensor", [t_dn], [t_dn], out=bg[:].rearrange("p a b -> p (a b)"),
              in0=bg[:].rearrange("p a b -> p (a b)"), in1=beta[:].rearrange("p a b -> p (a b)"), op=ALU.mult)
            V("tensor_scalar", [t_dn], [t_dn], out=nbeta[:].rearrange("p a b -> p (a b)"),
              in0=beta[:].rearrange("p a b -> p (a b)"), scalar1=-1.0, scalar2=None, op0=ALU.mult)
            fw.flush()

        with ExitStack() as ph:
            wh = [sb(ph, "wh%d" % m, [128, 8, 128], BF16) for m in range(4)]
            t_wh = Ts(4)
            convw = sb(ph, "convw", [128, 24, 4], F32)
            onormg = sb(ph, "onormg", [128, 1], F32)
            t_cw = T()
            xc = sb(ph, "xc", [128, 3 + L], F32)
            yc = sb(ph, "yc", [128, L], F32)
            sq = sb(ph, "sq", [128, L], BF16)
            qT = sb(ph, "qT", [128, L], BF16)
            kT = sb(ph, "kT", [128, L], BF16)
            vsT = sb(ph, "vsT", [128, L], BF16)
            zs = sb(ph, "zs", [128, L], BF16)
            obh = sb(ph, "obh", [128, L], BF16)
            t_xc, t_yc, t_sq, t_qT, t_kT, t_vsT, t_zs, t_obh = Ts(8)
            kbg = sb(ph, "kbg", [128, NT, 128], BF16)
            ktl = sb(ph, "ktl", [128, NT, 128], BF16)
            vb = sb(ph, "vb", [128, NT, 128], BF16)
            TTf = sb(ph, "TTf", [128, NT, 128], BF16)
            nwdT = sb(ph, "nwdT", [128, NT, 128], BF16)
            qkT = sb(ph, "qkT", [128, NT, 128], BF16)
            qdT = sb(ph, "qdT", [128, NT, 128], BF16)
            t_kbg, t_ktl, t_vb, t_TTf, t_nwdT, t_qkT, t_qdT = Ts(7)
            Pm = [sb(ph, "Pm%d" % i, [128, 8, 128], BF16) for i in range(2)]
            Qm = [sb(ph, "Qm%d" % i, [128, 8, 128], BF16) for i in range(2)]
            TTm = [sb(ph, "TTm%d" % i, [128, 8, 128], BF16) for i in range(2)]
            IPm = sb(ph, "IPm", [128, 8, 128], BF16)
            t_Pm, t_Qm, t_TTm = Ts(2), Ts(2), Ts(2)
            t_IPm = T()

            def f2(name):
                return [sb(ph, "%s%d" % (name, i), [128, 128], F32) for i in range(2)]
            gBn, Dm, dec, W1, DmT, decT, W2, eGb = [f2(nm) for nm in ("gBn", "Dm", "dec", "W1", "DmT", "decT", "W2", "eGb")]
            t_gBn, t_Dm, t_dec, t_W1, t_DmT, t_decT, t_W2, t_eGb = [Ts(2) for _ in range(8)]
            S = sb(ph, "S", [128, 128], F32)
            S_bf = sb(ph, "S_bf", [128, 128], BF16)
            u_bf = sb(ph, "u_bf", [128, 128], BF16)
            on_bf = sb(ph, "on_bf", [128, 128], BF16)
            jk = sb(ph, "jk", [128, 128], F32)
            st = sb(ph, "st", [128, 8], F32)
            t_S, t_Sbf, t_ubf, t_on, t_jk, t_st = Ts(6)
            B = [pst(ph, "B%d" % i, [128, 512], F32) for i in range(6)]
            H = [pst(ph, "H%d" % i, [128, 8, 128], BF16) for i in range(2)]
            t_B, t_H = Ts(6, True), Ts(2, True)
            ident8t = sb(ph, "ident8t", [128, 8, 128], F32)
            for j in range(8):
                G("tensor_copy", [t_const], [t_const], out=ident8t[:, j, :], in_=ident_f[:])
            ident4 = ident8t[:, 0:4, :]
            ident8 = ident8t[:]

            def b4(ap):
                return ap.rearrange("p (a b) -> p a b", a=4)

            bigL = sb(ph, "bigL", [128, 128], F32)
            nbigT = sb(ph, "nbigT", [128, 128], F32)
            V("tensor_scalar", [t_const], [t_const], out=bigL[:], in0=lstr_f[:], scalar1=30000.0, scalar2=30000.0,
              op0=ALU.mult, op1=ALU.subtract)
            V("tensor_scalar", [t_const], [t_const], out=bigL[:], in0=bigL[:], scalar1=-1.0, scalar2=None,
              op0=ALU.mult)
            V("tensor_scalar", [t_const], [t_const], out=nbigT[:], in0=lstr_f[:], scalar1=-30000.0, scalar2=None,
              op0=ALU.mult)
            fw.dma(convw[:], conv_d, writes=[t_cw])
            fw.dma(onormg[:], ong_d, writes=[t_cw])
            V("memset", [], [t_xc], ap=xc[:, 0:3], constant=0.0)

            for h in range(8):
                for m, base in enumerate((C_QB, C_KB, C_VB, C_Z)):
                    wload3(wh[m][:], w_in_v[:, :, base + h * 128:base + (h + 1) * 128], t_wh[m], 8, 128)
                for m in range(4):
                    for tb in range(4):
                        bk = (m * 4 + tb) % 2
                        tsl = slice(tb * 512, (tb + 1) * 512)
                        for kc in range(8):
                            mm(B[bk][:], wh[m][:, kc, :], hT[:, kc, tsl], kc == 0, kc == 7,
                               [t_wh[m]] + t_hT[tb * 4:(tb + 1) * 4], [t_B[bk]])
                        if m == 3:
                            A("activation", [t_B[bk]], [t_zs], out=zs[:, tsl], in_=B[bk][:], func=AF.Silu)
                        else:
                            A("copy", [t_B[bk]], [t_xc], out=xc[:, 3 + tb * 512:3 + (tb + 1) * 512], in_=B[bk][:])
                    if m == 3:
                        continue
                    cb = m * 8 + h
                    V("tensor_scalar", [t_xc, t_cw], [t_yc], out=yc[:], in0=xc[:, 0:L], scalar1=convw[:, cb, 0:1],
                      scalar2=None, op0=ALU.mult)
                    for j in range(1, 4):
                        V("scalar_tensor_tensor", [t_xc, t_cw, t_yc], [t_yc], out=yc[:], in0=xc[:, j:j + L],
                          scalar=convw[:, cb, j:j + 1], in1=yc[:], op0=ALU.mult, op1=ALU.add)
                    if m == 2:
                        A("activation", [t_yc], [t_vsT], out=vsT[:], in_=yc[:], func=AF.Silu)
                        continue
                    dst, t_dst = (qT, t_qT) if m == 0 else (kT, t_kT)
                    A("activation", [t_yc], [t_xc], out=xc[:, 3:3 + L], in_=yc[:], func=AF.Silu)
                    A("activation", [t_xc], [t_sq], out=sq[:], in_=xc[:, 3:3 + L], func=AF.Square)
                    sc_ = 128.0 if m == 0 else 1.0
                    for tb in range(4):
                        tsl = slice(tb * 512, (tb + 1) * 512)
                        mm(B[2][:], ones_b[:], sq[:, tsl], True, True, [t_const, t_sq], [t_B[2]])
                        A("activation", [t_B[2]], [t_yc], out=yc[:, tsl], in_=B[2][:], func=AF.Ln, scale=sc_,
                          bias=sc_ * EPS)
                    A("activation", [t_yc], [t_yc], out=yc[:], in_=yc[:], func=AF.Exp, scale=-0.5)
                    V("tensor_tensor", [t_xc, t_yc], [t_dst], out=dst[:], in0=xc[:, 3:3 + L], in1=yc[:], op=ALU.mult)
                if DEBUG_FLUSH and h == 0:
                    fw.flush()
                for g2 in range(2):
                    gs = slice(g2 * 8, (g2 + 1) * 8)
                    for j in range(8):
                        n = g2 * 8 + j
                        P("transpose", [t_kT, t_const], [t_H[0]], out=H[0][:, j, :], in_=kT[:, n * 128:(n + 1) * 128],
                          identity=ident_b[:])
                    V("tensor_tensor", [t_H[0], t_dn], [t_kbg], out=kbg[:, gs, :], in0=H[0][:],
                      in1=bg[:, gs, h].unsqueeze(2).to_broadcast([128, 8, 128]), op=ALU.mult)
                    V("tensor_tensor", [t_H[0], t_dn], [t_ktl], out=ktl[:, gs, :], in0=H[0][:],
                      in1=eTail[:, gs, h].unsqueeze(2).to_broadcast([128, 8, 128]), op=ALU.mult)
                    for j in range(8):
                        n = g2 * 8 + j
                        P("transpose", [t_vsT, t_const], [t_H[1]], out=H[1][:, j, :], in_=vsT[:, n * 128:(n + 1) * 128],
                          identity=ident_b[:])
                    V("tensor_tensor", [t_H[1], t_dn], [t_vb], out=vb[:, gs, :], in0=H[1][:],
                      in1=beta[:, gs, h].unsqueeze(2).to_broadcast([128, 8, 128]), op=ALU.mult)
                if DEBUG_FLUSH and h == 0:
                    fw.flush()
                for half in range(2):
                    for j in range(8):
                        n = half * 8 + j
                        i2 = n % 2
                        ns = slice(n * 128, (n + 1) * 128)
                        Gcol = Gc[:, n, h:h + 1]
                        V("tensor_scalar", [t_const, t_dn], [t_gBn[i2]], out=gBn[i2][:], in0=ones_f[:],
                          scalar1=gdec[:, n, h:h + 1], scalar2=None, op0=ALU.mult)
                        gb_ps = B[3 + i2][:, 0:128]
                        mm(gb_ps, gBn[i2][:], uincl_f[:], True, True, [t_gBn[i2], t_const], [t_B[3 + i2]])
                        V("scalar_tensor_tensor", [t_B[3 + i2], t_dn, t_const], [t_Dm[i2]], out=Dm[i2][:], in0=gb_ps,
                          scalar=Gcol, in1=bigL[:], op0=ALU.subtract, op1=ALU.max)
                        A("activation", [t_Dm[i2]], [t_W1[i2]], out=W1[i2][:], in_=Dm[i2][:], func=AF.Exp, scale=-1.0)
                        V("scalar_tensor_tensor", [t_B[3 + i2], t_dn, t_const], [t_DmT[i2]], out=DmT[i2][:], in0=gb_ps,
                          scalar=Gcol, in1=nbigT[:], op0=ALU.subtract, op1=ALU.min)
                        A("activation", [t_DmT[i2]], [t_W2[i2]], out=W2[i2][:], in_=DmT[i2][:], func=AF.Exp)
                        A("activation", [t_B[3 + i2]], [t_eGb[i2]], out=eGb[i2][:], in_=gb_ps, func=AF.Exp)
                        G("tensor_tensor", [t_qT, t_eGb[i2]], [t_qdT], out=qdT[:, n, :], in0=qT[:, ns], in1=eGb[i2][:],
                          op=ALU.mult)
                        mm(B[i2][:, 0:128], kT[:, ns], kT[:, ns], True, True, [t_kT], [t_B[i2]])
                        mm(B[i2][:, 128:256], kT[:, ns], qT[:, ns], True, True, [t_kT, t_qT], [t_B[i2]])
                        V("scalar_tensor_tensor", [t_B[i2], t_dn, t_W1[i2]], [t_Pm[0]], out=Pm[0][:, j, :],
                          in0=B[i2][:, 0:128], scalar=nbeta[:, n, h:h + 1], in1=W1[i2][:], op0=ALU.mult, op1=ALU.mult)
                        V("tensor_tensor", [t_B[i2], t_W2[i2]], [t_qkT], out=qkT[:, n, :], in0=B[i2][:, 128:256],
                          in1=W2[i2][:], op=ALU.mult)
                        P("transpose", [t_Pm[0], t_const], [t_H[1]], out=H[1][:, j, :], in_=Pm[0][:, j, :],
                          identity=ident_b[:])
                    if DEBUG_FLUSH and h == 0 and half == 0:
                        fw.flush()
                    A("copy", [t_H[1]], [t_Qm[0]], out=Qm[0][:], in_=H[1][:])
                    V("tensor_tensor", [t_H[1], t_const], [t_TTm[0]], out=TTm[0][:], in0=H[1][:], in1=ident8, op=ALU.add)
                    cur = 0
                    for k in range(6):
                        nxt = 1 - cur
                        for g4 in range(2):
                            cs = slice(g4 * 4, g4 * 4 + 4)
                            for j in range(4):
                                c = g4 * 4 + j
                                mm(B[2 + g4][:, j * 128:(j + 1) * 128], Qm[cur][:, c, :], Pm[cur][:, c, :], True, True,
                                   [t_Qm[cur], t_Pm[cur]], [t_B[2 + g4]])
                            A("copy", [t_B[2 + g4]], [t_Pm[nxt]], out=Pm[nxt][:, cs, :], in_=b4(B[2 + g4][:]))
                            V("tensor_tensor", [t_B[2 + g4], t_const], [t_IPm], out=IPm[:, cs, :], in0=b4(B[2 + g4][:]),
                              in1=ident4, op=ALU.add)
                            if k < 5:
                                for j in range(4):
                                    c = g4 * 4 + j
                                    mm(B[4 + g4][:, j * 128:(j + 1) * 128], Pm[cur][:, c, :], Qm[cur][:, c, :], True,
                                       True, [t_Qm[cur], t_Pm[cur]], [t_B[4 + g4]])
                                A("copy", [t_B[4 + g4]], [t_Qm[nxt]], out=Qm[nxt][:, cs, :], in_=b4(B[4 + g4][:]))
                            for j in range(4):
                                c = g4 * 4 + j
                                mm(B[g4][:, j * 128:(j + 1) * 128], IPm[:, c, :], TTm[cur][:, c, :], True, True,
                                   [t_IPm, t_TTm[cur]], [t_B[g4]])
                            if k == 5:
                                V("tensor_copy", [t_B[g4]], [t_TTf],
                                  out=TTf[:, half * 8 + g4 * 4:half * 8 + g4 * 4 + 4, :], in_=b4(B[g4][:]))
                            else:
                                V("tensor_copy", [t_B[g4]], [t_TTm[nxt]], out=TTm[nxt][:, cs, :], in_=b4(B[g4][:]))
                        cur = nxt
                        if DEBUG_FLUSH and h == 0 and half == 0:
                            fw.flush()
                    for g4 in range(2):
                        n0 = half * 8 + g4 * 4
                        for j in range(4):
                            mm(B[4 + g4][:, j * 128:(j + 1) * 128], kbg[:, n0 + j, :], TTf[:, n0 + j, :], True, True,
                               [t_kbg, t_TTf], [t_B[4 + g4]])
                        V("tensor_scalar", [t_B[4 + g4]], [t_nwdT], out=nwdT[:, n0:n0 + 4, :], in0=b4(B[4 + g4][:]),
                          scalar1=-1.0, scalar2=None, op0=ALU.mult)
                if DEBUG_FLUSH and h == 0:
                    fw.flush()
                for n in range(NT):
                    ns = slice(n * 128, (n + 1) * 128)
                    mm(B[0][:, 0:128], TTf[:, n, :], vb[:, n, :], True, n == 0, [t_TTf, t_vb], [t_B[0]])
                    if n > 0:
                        mm(B[0][:, 0:128], nwdT[:, n, :], S_bf[:], False, True, [t_nwdT, t_Sbf], [t_B[0]])
                    A("copy", [t_B[0]], [t_ubf], out=u_bf[:], in_=B[0][:, 0:128])
                    if n > 0:
                        mm(B[1][:, 0:128], qdT[:, n, :], S_bf[:], True, False, [t_qdT, t_Sbf], [t_B[1]])
                    mm(B[1][:, 0:128], qkT[:, n, :], u_bf[:], n == 0, True, [t_qkT, t_ubf], [t_B[1]])
                    if n < NT - 1:
                        mm(B[2][:, 0:128], ktl[:, n, :], u_bf[:], True, True, [t_ktl, t_ubf], [t_B[2]])
                        if n == 0:
                            V("tensor_copy", [t_B[2]], [t_Sbf], out=S_bf[:], in_=B[2][:, 0:128])
                            V("tensor_copy", [t_B[2]], [t_S], out=S[:], in_=B[2][:, 0:128])
                        else:
                            V("scalar_tensor_tensor", [t_S, t_dn, t_B[2]], [t_Sbf], out=S_bf[:], in0=S[:],
                              scalar=aTail[:, n, h:h + 1], in1=B[2][:, 0:128], op0=ALU.mult, op1=ALU.add)
                            V("scalar_tensor_tensor", [t_S, t_dn, t_B[2]], [t_S], out=S[:], in0=S[:],
                              scalar=aTail[:, n, h:h + 1], in1=B[2][:, 0:128], op0=ALU.mult, op1=ALU.add)
                    A("activation", [t_B[1]], [t_jk, t_st], out=jk[:], in_=B[1][:, 0:128], func=AF.Square,
                      accum_out=st[:, 0:1])
                    rstd(t_st, st[:, 0:1], st[:, 1:2], st[:, 3:4], 1.0 / 128, EPS)
                    V("tensor_scalar", [t_B[1], t_st], [t_on], out=on_bf[:], in0=B[1][:, 0:128], scalar1=st[:, 3:4],
                      scalar2=None, op0=ALU.mult)
                    P("transpose", [t_on, t_const], [t_H[0]], out=H[0][:, 0, :], in_=on_bf[:], identity=ident_b[:])
                    V("scalar_tensor_tensor", [t_H[0], t_cw, t_zs], [t_obh], out=obh[:, ns], in0=H[0][:, 0, :],
                      scalar=onormg[:, 0:1], in1=zs[:, ns], op0=ALU.mult, op1=ALU.mult)
                fw.dma(ob_d[h], obh[:], reads=[t_obh], writes=[t_obd])
                if DEBUG_FLUSH and h == 0:
                    fw.flush()
            fw.flush()

        with ExitStack() as ph:
            Mh = sb(ph, "Mh", [128, 2, 16, 256], BF16)
            wiq = sb(ph, "wiq", [128, 2, 512], BF16)
            wuvp = sb(ph, "wuvp", [128, 2, 16, 128], BF16)
            t_wt = T()
            F01 = [pst(ph, "F0", [128, 512], F32), pst(ph, "F1", [128, 512], F32)]
            FQ = pst(ph, "FQ", [128, 1024], F32)
            F45 = [pst(ph, "F4", [128, 512], F32), pst(ph, "F5", [128, 512], F32)]
            H0 = pst(ph, "AH0", [128, 8, 128], BF16)
            t_F01, t_FQ, t_F45 = Ts(2, True), Ts(2, True), Ts(2, True)
            t_H0 = T(True)
            with ExitStack() as ph2:
                wuq = sb(ph2, "wuq", [128, 2, 1024], BF16)
                wuqT = sb(ph2, "wuqT", [128, 8, 256], BF16)
                wuk = sb(ph2, "wuk", [128, 8, 256], BF16)
                for rc in range(2):
                    wload(wuq[:, rc, :], w_uq[rc * 128:(rc + 1) * 128, :], t_wt, 1024)
                    wload(wiq[:, rc, :], w_iq[rc * 128:(rc + 1) * 128, :], t_wt, 512)
                wukv = w_uk.rearrange("(hp two) d r -> (two d) hp r", two=2)
                for j in range(2):
                    wload3(wuk[:, j * 4:(j + 1) * 4, :], wukv[:, j * 4:(j + 1) * 4, :], t_wt, 4, 256)
                G("memset", [], [t_wt], ap=wuvp[:], constant=0.0)
                wuvv = w_uv.rearrange("h (rc r) d -> r rc h d", rc=2)
                for rc in range(2):
                    i = stg_i[0] % 3
                    stg_i[0] += 1
                    sv = stg[i][:, 0:1024].rearrange("p (h d) -> p h d", h=16)
                    fw.dma(sv, wuvv[:, rc, :, :], writes=[t_stg[i]])
                    for h in range(16):
                        G("tensor_copy", [t_stg[i]], [t_wt], out=wuvp[:, rc, h, (h % 2) * 64:(h % 2) * 64 + 64],
                          in_=sv[:, h, :])
                for rc in range(2):
                    for cb in range(8):
                        P("transpose", [t_wt, t_const], [t_H0], out=H0[:, cb, :],
                          in_=wuq[:, rc, cb * 128:(cb + 1) * 128], identity=ident_b[:])
                    V("tensor_copy", [t_H0], [t_wt], out=wuqT[:, :, rc * 128:(rc + 1) * 128], in_=H0[:])
                for h in range(16):
                    hp, two = h // 2, h % 2
                    ps_ = slice(two * 64, two * 64 + 64)
                    for rc in range(2):
                        mm(F01[rc][:, 0:256], wuqT[ps_, hp, rc * 128:(rc + 1) * 128], wuk[ps_, hp, :], True, True,
                           [t_wt], [t_F01[rc]])
                        A("copy", [t_F01[rc]], [t_wt], out=Mh[:, rc, h, :], in_=F01[rc][:, 0:256])
                fw.flush()

            sc = sb(ph, "sc", [128, L], F32)
            jnk = sb(ph, "jnk", [128, L], BF16)
            biasm = sb(ph, "biasm", [128, L], BF16)
            biasT = sb(ph, "biasT", [128, NT, 128], BF16)
            qlT = sb(ph, "qlT", [128, 2, 16, 128], BF16)
            qiT = sb(ph, "qiT", [64, 8, 128], BF16)
            rl = [sb(ph, "rl%d" % i, [128, 512], F32) for i in range(2)]
            pt = [sb(ph, "pt%d" % i, [128, 512], BF16) for i in range(2)]
            rden = sb(ph, "rden", [128, 512], F32)
            onT = sb(ph, "onT", [128, 2, 512], BF16)
            bis = sb(ph, "bis", [128, 8], F32)
            dl = sb(ph, "dl", [128, 32], F32)
            crow = sb(ph, "crow", [128, 32], F32)
            oaq = [sb(ph, "oaq%d" % i, [128, 8, 128], BF16) for i in range(2)]
            t_sc, t_jnk, t_biasm, t_biasT, t_qlT, t_qiT, t_rden, t_onT, t_bis = Ts(9)
            t_rl, t_pt, t_oaq = Ts(2), Ts(2), Ts(2)
            for k in range(32):
                V("memset", [], [t_bis], ap=crow[:, k:k + 1], constant=float(2.0 ** (-k)))

            AX = pst(ph, "AX", [128, 512], F32)
            t_AX = T(True)
            qlT2 = [qlT, sb(ph, "qlT_b", [128, 2, 16, 128], BF16)]
            biasT2 = [biasT, sb(ph, "biasT_b", [128, NT, 128], BF16)]
            t_qlT2, t_biasT2 = Ts(2), Ts(2)

            def stageA(qb):
                qs = slice(qb * 128, (qb + 1) * 128)
                Tk = (qb + 1) * 128
                qlT_, t_qlT_ = qlT2[qb % 2], t_qlT2[qb % 2]
                biasT_, t_biasT_ = biasT2[qb % 2], t_biasT2[qb % 2]
                for half in range(2):
                    for hh in range(4):
                        hi = half * 4 + hh
                        for rc in range(2):
                            mm(AX[0:64, hh * 128:(hh + 1) * 128], wiq[:, rc, hi * 64:(hi + 1) * 64], cqT[:, rc, qs],
                               rc == 0, rc == 1, [t_wt, t_cqT[qb]], [t_AX])
                    A("copy", [t_AX], [t_qiT], out=qiT[:, half * 4:(half + 1) * 4, :],
                      in_=AX[0:64, :].rearrange("p (a b) -> p a b", a=4))
                    yield
                for rco in range(2):
                    for hg in range(4):
                        for hl in range(4):
                            h = hg * 4 + hl
                            for rci in range(2):
                                mm(AX[:, hl * 128:(hl + 1) * 128], Mh[:, rci, h, rco * 128:(rco + 1) * 128],
                                   cqT[:, rci, qs], rci == 0, rci == 1, [t_wt, t_cqT[qb]], [t_AX])
                        yield
                        V("tensor_copy", [t_AX], [t_qlT_], out=qlT_[:, rco, hg * 4:(hg + 1) * 4, :],
                          in_=AX[:].rearrange("p (a b) -> p a b", a=4))
                for kg in range(qb // 4 + 1):
                    ncol = min(512, Tk - kg * 512)
                    ks = slice(kg * 512, kg * 512 + ncol)
                    kts = [t_kiT[j] for j in range(kg * 4, kg * 4 + ncol // 128)]
                    for hi in range(8):
                        b2 = hi % 2
                        mm(AX[:, 0:ncol], qiT[:, hi, :], kiT[:, ks], True, True, [t_qiT] + kts, [t_AX])
                        yield
                        A("activation", [t_AX, t_w], [t_rl[b2]], out=rl[b2][:, 0:ncol], in_=AX[:, 0:ncol],
                          func=AF.Relu, scale=absw[:, qb, hi:hi + 1])
                        if hi == 0:
                            V("tensor_scalar", [t_rl[b2], t_w], [t_sc], out=sc[:, ks], in0=rl[b2][:, 0:ncol],
                              scalar1=sgnw[:, qb, hi:hi + 1], scalar2=None, op0=ALU.mult)
                        else:
                            V("scalar_tensor_tensor", [t_rl[b2], t_w, t_sc], [t_sc], out=sc[:, ks],
                              in0=rl[b2][:, 0:ncol], scalar=sgnw[:, qb, hi:hi + 1], in1=sc[:, ks], op0=ALU.mult,
                              op1=ALU.add)
                if qb >= 2:
                    V("tensor_reduce", [t_sc], [t_bis], out=bis[:, 0:1], in_=sc[:, 0:Tk], axis=AX_.X, op=ALU.max,
                      apply_absolute_value=True)
                    V("tensor_scalar", [t_bis], [t_bis], out=dl[:], in0=crow[:], scalar1=bis[:, 0:1], scalar2=None,
                      op0=ALU.mult)
                    V("memset", [], [t_bis], ap=bis[:, 1:2], constant=0.0)
                G("affine_select", [t_sc], [t_sc], out=sc[:, qs], in_=sc[:, qs], pattern=[[-1, 128]],
                  compare_op=ALU.is_ge, fill=-1e30, base=0, channel_multiplier=1)
                yield
                if qb >= 2:
                    cur = 1
                    for k in range(NBIS):
                        nxt = 3 - cur
                        V("tensor_scalar", [t_sc, t_bis], [t_jnk, t_bis], out=jnk[:, 0:Tk], in0=sc[:, 0:Tk],
                          scalar1=bis[:, cur:cur + 1], scalar2=0.0, op0=ALU.is_ge, op1=ALU.add,
                          accum_out=bis[:, 3:4])
                        V("tensor_scalar", [t_bis], [t_bis], out=bis[:, 4:5], in0=bis[:, 3:4], scalar1=255.5,
                          scalar2=0.5, op0=ALU.is_gt, op1=ALU.subtract)
                        V("scalar_tensor_tensor", [t_bis], [t_bis], out=bis[:, nxt:nxt + 1], in0=bis[:, 4:5],
                          scalar=dl[:, k:k + 1], in1=bis[:, cur:cur + 1], op0=ALU.mult, op1=ALU.add)
                        cur = nxt
                        yield
                    V("tensor_tensor", [t_bis], [t_bis], out=bis[:, 5:6], in0=bis[:, cur:cur + 1],
                      in1=dl[:, NBIS:NBIS + 1], op=ALU.subtract)
                else:
                    V("memset", [], [t_bis], ap=bis[:, 5:6], constant=-1e29)
                V("tensor_scalar", [t_sc, t_bis], [t_biasm], out=biasm[:, 0:Tk], in0=sc[:, 0:Tk], scalar1=bis[:, 5:6],
                  scalar2=NEG, op0=ALU.is_lt, op1=ALU.mult)
                yield
                for kb0 in range(0, qb + 1, 8):
                    nk = min(8, qb + 1 - kb0)
                    for j in range(nk):
                        kb = kb0 + j
                        P("transpose", [t_biasm, t_const], [t_H0], out=H0[:, j, :],
                          in_=biasm[:, kb * 128:(kb + 1) * 128], identity=ident_b[:])
                    A("copy", [t_H0], [t_biasT_], out=biasT_[:, kb0:kb0 + nk, :], in_=H0[:, 0:nk, :])
                    yield

            def stageE(qb, filler):
                qs = slice(qb * 128, (qb + 1) * 128)
                qlT_, t_qlT_ = qlT2[qb % 2], t_qlT2[qb % 2]
                biasT_, t_biasT_ = biasT2[qb % 2], t_biasT2[qb % 2]
                psO = [FQ[:, 0:512], FQ[:, 512:1024]]
                psD = F45[0]
                psOA = F45[1]
                oq = oaq[qb % 2]
                t_oq = t_oaq[qb % 2]

                def fill(n):
                    for _ in range(n):
                        if filler is not None:
                            next(filler, None)

                def qk_scores(hg, kb):
                    b2 = kb % 2
                    kbs = slice(kb * 128, (kb + 1) * 128)
                    for rc in range(2):
                        mm(F01[b2][:], ckvT[:, rc, kbs],
                           qlT_[:, rc, hg * 4:(hg + 1) * 4, :].rearrange("p a b -> p (a b)"),
                           rc == 0, False, [t_ckvT[kb], t_qlT_], [t_F01[b2]])
                    mm(F01[b2][:], ident_b[:], biasT_[:, kb:kb + 1, :].to_broadcast([128, 4, 128]), False, True,
                       [t_const, t_biasT_], [t_F01[b2]])

                for hg in range(4):
                    qk_scores(hg, 0)
                    for kb in range(qb + 1):
                        b2 = kb % 2
                        if kb < qb:
                            qk_scores(hg, kb + 1)
                        A("activation", [t_F01[b2]], [t_pt[b2]], out=pt[b2][:], in_=F01[b2][:], func=AF.Exp,
                          scale=0.125)
                        for rc in range(2):
                            mm(psO[rc], ckv1[:, kb, rc * 128:(rc + 1) * 128], pt[b2][:], kb == 0, kb == qb,
                               [t_ckv1[kb], t_pt[b2]], [t_FQ[rc]])
                        mm(psD[:], ones_b[:], pt[b2][:], kb == 0, kb == qb, [t_const, t_pt[b2]], [t_F45[0]])
                        fill(3)
                    V("reciprocal", [t_F45[0]], [t_rden], out=rden[:], in_=psD[:])
                    for rc in range(2):
                        V("tensor_tensor", [t_FQ[rc], t_rden], [t_onT], out=onT[:, rc, :], in0=psO[rc], in1=rden[:],
                          op=ALU.mult)
                    for hpl in range(2):
                        hp = hg * 2 + hpl
                        k = 0
                        for two in range(2):
                            h = hp * 2 + two
                            hl = h - hg * 4
                            for rc in range(2):
                                mm(psOA[:, hpl * 128:(hpl + 1) * 128], wuvp[:, rc, h, :],
                                   onT[:, rc, hl * 128:(hl + 1) * 128], k == 0, k == 3, [t_wt, t_onT], [t_F45[1]])
                                k += 1
                    A("copy", [t_F45[1]], [t_oq], out=oq[:, hg * 2:hg * 2 + 2, :],
                      in_=psOA[:, 0:256].rearrange("p (a b) -> p a b", a=2))
                fw.dma(oa_v[:, :, qs], oq[:], reads=[t_oq], writes=[t_oad])

            for _ in stageA(0):
                pass
            for qb in range(NT):
                gen = stageA(qb + 1) if qb + 1 < NT else None
                stageE(qb, gen)
                if gen is not None:
                    for _ in gen:
                        pass
            fw.flush()
        att.close()

        with ExitStack() as ph:
            PA = sb(ph, "PA", [128, 8, D], BF16)
            PB = sb(ph, "PB", [128, 8, D], BF16)
            WO = sb(ph, "WO", [128, 8, D], BF16)
            t_PA, t_PB, t_WO = Ts(3)
            wga = sb(ph, "wga", [128, 8, D], BF16)
            wgb = sb(ph, "wgb", [128, 8, D], BF16)
            t_wga, t_wgb = T(), T()
            oab = sb(ph, "oab", [128, 8, 512], BF16)
            obb = sb(ph, "obb", [128, 8, 512], BF16)
            t_oab, t_obb = T(), T()
            sgA = sb(ph, "sgA", [128, 512], F32)
            sgB = sb(ph, "sgB", [128, 512], F32)
            tA = sb(ph, "tA", [128, 512], F32)
            tB = sb(ph, "tB", [128, 512], F32)
            t_sgA, t_sgB, t_tA, t_tB = Ts(4)
            mT = sb(ph, "mT", [128, 8, 512], BF16)
            t_mT = T()
            xt2 = [sb(ph, "xt2_%d" % i, [128, D], F32) for i in range(2)]
            x2t = [sb(ph, "x2t_%d" % i, [128, D], F32) for i in range(2)]
            t_xt2, t_x2t = Ts(2), Ts(2)
            B = [pst(ph, "MB%d" % i, [128, 512], F32) for i in range(6)]
            t_B = Ts(6, True)
            cast_engs[:] = ["scalar", "vector"]
            for kc in range(8):
                ks_ = slice(kc * 128, (kc + 1) * 128)
                wload(PA[:, kc, :], w_ba[ks_, :], t_PA, 1024)
                wload(PB[:, kc, :], w_bb[ks_, :], t_PB, 1024)
                wload(WO[:, kc, :], w_out[ks_, :], t_WO, 1024)
                wload(wga[:, kc, :], w_in_v[:, kc, C_GA:C_GA + D], t_wga, 1024)
                wload(wgb[:, kc, :], w_in_v[:, kc, C_GB:C_GB + D], t_wgb, 1024)
            cnt = 0
            for tb in range(4):
                tsl = slice(tb * 512, (tb + 1) * 512)
                fw.dma(oab[:], oa_v[:, :, tsl], reads=[t_oad], writes=[t_oab])
                fw.dma(obb[:], ob_v[:, :, tsl], reads=[t_obd], writes=[t_obb])
                for nc_ in range(8):
                    i2 = cnt % 2
                    cnt += 1
                    ncs = slice(nc_ * 128, (nc_ + 1) * 128)
                    hts = t_hT[tb * 4:(tb + 1) * 4]
                    for kc in range(8):
                        mm(B[0][:], wga[:, kc, ncs], hT[:, kc, tsl], kc == 0, kc == 7, [t_wga] + hts, [t_B[0]])
                    for kc in range(8):
                        mm(B[1][:], wgb[:, kc, ncs], hT[:, kc, tsl], kc == 0, kc == 7, [t_wgb] + hts, [t_B[1]])
                    A("activation", [t_B[0]], [t_sgA], out=sgA[:], in_=B[0][:], func=AF.Sigmoid)
                    A("activation", [t_B[1]], [t_sgB], out=sgB[:], in_=B[1][:], func=AF.Sigmoid)
                    for hp in range(8):
                        mm(B[2][:], PA[:, hp, ncs], oab[:, hp, :], hp == 0, hp == 7, [t_PA, t_oab], [t_B[2]])
                    for hh in range(8):
                        mm(B[3][:], PB[:, hh, ncs], obb[:, hh, :], hh == 0, hh == 7, [t_PB, t_obb], [t_B[3]])
                    V("tensor_tensor", [t_B[2], t_sgA], [t_tA], out=tA[:], in0=B[2][:], in1=sgA[:], op=ALU.mult)
                    V("tensor_tensor", [t_B[3], t_sgB], [t_tB], out=tB[:], in0=B[3][:], in1=sgB[:], op=ALU.mult)
                    G("tensor_tensor", [t_tA, t_tB], [t_mT], out=mT[:, nc_, :], in0=tA[:], in1=tB[:], op=ALU.add)
                for j in range(4):
                    tt = tb * 4 + j
                    i2 = tt % 2
                    rows = slice(tt * 128, (tt + 1) * 128)
                    fw.dma(xt2[i2][:], x_d[rows, :], writes=[t_xt2[i2]])
                    for half in range(2):
                        hs = slice(half * 512, (half + 1) * 512)
                        for nc_ in range(8):
                            mm(B[4 + half][:], mT[:, nc_, j * 128:(j + 1) * 128], WO[:, nc_, hs], nc_ == 0, nc_ == 7,
                               [t_mT, t_WO], [t_B[4 + half]])
                        V("tensor_tensor", [t_B[4 + half], t_xt2[i2]], [t_x2t[i2]], out=x2t[i2][:, hs],
                          in0=B[4 + half][:], in1=xt2[i2][:, hs], op=ALU.add)
                    fw.dma(x2_d[rows, :], x2t[i2][:], reads=[t_x2t[i2]], writes=[t_x2d])
            fw.flush()
        mix.close()

        with ExitStack() as ph:
            Wg = sb(ph, "Wg", [128, 8, DFF], BF16)
            Wu = sb(ph, "Wu", [128, 8, DFF], BF16)
            Wd = sb(ph, "Wd", [128, NFC, D], BF16)
            t_Wg, t_Wu, t_Wd = Ts(3)
            gBf = sb(ph, "gBf", [128, 8, 128], F32)
            ffng = sb(ph, "ffng", [128, 8], F32)
            fing = sb(ph, "fing", [128, D], F32)
            t_fg = T()
            x2t = [sb(ph, "fx2t_%d" % i, [128, D], F32) for i in range(2)]
            t_x2t = Ts(2)
            x3 = sb(ph, "x3", [128, D], F32)
            yo = sb(ph, "yo", [128, D], F32)
            xs = sb(ph, "fxs", [128, D], BF16)
            ss = sb(ph, "fss", [128, 8], F32)
            fs = sb(ph, "ffs", [128, 8], F32)
            h2T = sb(ph, "h2T", [128, 8, 256], BF16)
            act = sb(ph, "act", [128, NFC, 256], BF16)
            sg = [sb(ph, "sg%d" % i, [128, 256], F32) for i in range(2)]
            t_x3, t_yo, t_fs, t_h2T, t_act = Ts(5)
            t_sg = Ts(2)
            pT = pst(ph, "fpT", [128, 8, 128], BF16)
            B = [pst(ph, "FB%d" % i, [128, 512], F32) for i in range(6)]
            t_B = Ts(6, True)
            res = (yo, t_yo, ss, T(), xs, T(), pT, T(True))
            fw.dma(ffng[:], ffng_d, writes=[t_fg])
            fw.dma(fing[:], fing_d, writes=[t_fg])
            for kc in range(8):
                V("tensor_scalar", [t_const, t_fg], [t_fg], out=gBf[:, kc, :], in0=ones_f[:],
                  scalar1=ffng[:, kc:kc + 1], scalar2=None, op0=ALU.mult)
            for kc in range(8):
                ks_ = slice(kc * 128, (kc + 1) * 128)
                for c0 in (0, 1024, 2048):
                    w_ = min(1024, DFF - c0)
                    wload(Wg[:, kc, c0:c0 + w_], w_gate[ks_, c0:c0 + w_], t_Wg, w_)
                    wload(Wu[:, kc, c0:c0 + w_], w_up[ks_, c0:c0 + w_], t_Wu, w_)
            for fc in range(NFC):
                wload(Wd[:, fc, :], w_down[fc * 128:(fc + 1) * 128, :], t_Wd, 1024)
            for tb2 in range(8):
                for j in range(2):
                    tt = tb2 * 2 + j
                    fw.dma(x2t[j][:], x2_d[tt * 128:(tt + 1) * 128, :], reads=[t_x2d], writes=[t_x2t[j]])
                    rmsnorm_T("ffn", x2t[j][:], t_x2t[j], gBf, h2T, t_h2T, j * 128, res)
                for fc in range(NFC):
                    bk = fc % 2
                    fcs = slice(fc * 128, (fc + 1) * 128)
                    for kc in range(8):
                        mm(B[bk][:, 0:256], Wg[:, kc, fcs], h2T[:, kc, :], kc == 0, kc == 7, [t_Wg, t_h2T], [t_B[bk]])
                    for kc in range(8):
                        mm(B[bk][:, 256:512], Wu[:, kc, fcs], h2T[:, kc, :], kc == 0, kc == 7, [t_Wu, t_h2T],
                           [t_B[bk]])
                    A("activation", [t_B[bk]], [t_sg[bk]], out=sg[bk][:], in_=B[bk][:, 0:256], func=AF.Silu)
                    V("tensor_tensor", [t_B[bk], t_sg[bk]], [t_act], out=act[:, fc, :], in0=B[bk][:, 256:512],
                      in1=sg[bk][:], op=ALU.mult)
                for j in range(2):
                    tt = tb2 * 2 + j
                    for half in range(2):
                        bi = 2 + j * 2 + half
                        hs = slice(half * 512, (half + 1) * 512)
                        for fc in range(NFC):
                            mm(B[bi][:], act[:, fc, j * 128:(j + 1) * 128], Wd[:, fc, hs], fc == 0, fc == NFC - 1,
                               [t_act, t_Wd], [t_B[bi]])
                        V("tensor_tensor", [t_B[bi], t_x2t[j]], [t_x3], out=x3[:, hs], in0=B[bi][:],
                          in1=x2t[j][:, hs], op=ALU.add)
                    A("activation", [t_x3], [t_yo, t_fs], out=yo[:], in_=x3[:], func=AF.Square, accum_out=fs[:, 0:1])
                    rstd(t_fs, fs[:, 0:1], fs[:, 1:2], fs[:, 3:4], 1.0 / D, EPS)
                    V("scalar_tensor_tensor", [t_x3, t_fs, t_fg], [t_yo], out=yo[:], in0=x3[:], scalar=fs[:, 3:4],
                      in1=fing[:], op0=ALU.mult, op1=ALU.mult)
                    fw.dma(out_d[tt * 128:(tt + 1) * 128, :], yo[:], reads=[t_yo])
            fw.flush()
        print("bass ops recorded:", fw.nops)
    except _Stop:
        print("build truncated after flush", STOP_AFTER_FLUSH)
    return nc


_NC_CACHE = {}


def _rep(v, n=128):
    return np.ascontiguousarray(np.tile(np.asarray(v, np.float32).reshape(1, -1), (n, 1)))


def kernel(x, mix_norm_g, w_in, cq_norm_g, ckv_norm_g, w_uq, w_uk, w_uv, w_iq,
           kidx_ln_g, kidx_ln_b, w_branch_a, conv_w, a_log, dt_bias, onorm_g,
           w_branch_b, w_out, ffn_norm_g, w_gate, w_up, w_down, final_norm_g):
    f = lambda a: np.ascontiguousarray(np.asarray(a, dtype=np.float32))
    x = f(x)
    n = x.shape[0]
    if "nc" not in _NC_CACHE:
        _NC_CACHE["nc"] = build_nc()
    nc = _NC_CACHE["nc"]
    shared = {
        "w_in": f(w_in)[0], "w_uq": f(w_uq)[0], "w_uk": f(w_uk)[0], "w_uv": f(w_uv)[0], "w_iq": f(w_iq)[0],
        "w_branch_a": f(w_branch_a)[0], "w_branch_b": f(w_branch_b)[0], "w_out": f(w_out)[0],
        "w_gate": f(w_gate)[0], "w_up": f(w_up)[0], "w_down": f(w_down)[0],
        "mixg_col": np.ascontiguousarray(f(mix_norm_g)[0].reshape(8, 128).T),
        "ffng_col": np.ascontiguousarray(f(ffn_norm_g)[0].reshape(8, 128).T),
        "fing_row": _rep(final_norm_g),
        "cqg_row": _rep(f(cq_norm_g)[0]), "ckvg_row": _rep(f(ckv_norm_g)[0]),
        "klng_row": _rep(f(kidx_ln_g)[0]), "klnb_row": _rep(f(kidx_ln_b)[0]),
        "conv_col": np.ascontiguousarray(f(conv_w)[0].T.reshape(24, 128, 4).transpose(1, 0, 2)),
        "alog_row": _rep(f(a_log)[0]), "dtb_row": _rep(f(dt_bias)[0]),
        "onormg_col": np.ascontiguousarray(f(onorm_g)[0].reshape(128, 1)),
        "ident": np.eye(128, dtype=np.float32),
        "uincl": np.triu(np.ones((128, 128), np.float32)),
        "lstrict": np.tril(np.ones((128, 128), np.float32), -1),
    }
    in_maps = [dict(shared, x=x[i]) for i in range(n)]
    res = run_bass_kernel_spmd(nc, in_maps, core_ids=list(range(n)))
    out = np.stack([np.asarray(r["out"], dtype=np.float32) for r in res.results], axis=0)
    if DEBUG:
        kernel.debug = res.results
    return out
```

```python
from contextlib import ExitStack

import numpy as np
import concourse.bass as bass
import concourse.mybir as mybir
from concourse.bass_utils import run_bass_kernel_spmd

F32 = mybir.dt.float32
BF16 = mybir.dt.bfloat16
AF = mybir.ActivationFunctionType
ALU = mybir.AluOpType
AX_ = mybir.AxisListType

L = 2048
D = 1024
NT = 16
EPS = 1e-6
DFF = 2816
NFC = DFF // 128
INW = 6744
C_Q, C_KV, C_KI, C_WI = 0, 256, 512, 576
C_QB, C_KB, C_VB = 584, 1608, 2632
C_BETA, C_A = 3656, 3664
C_Z, C_GA, C_GB = 3672, 4696, 5720
IDX_SCALE = float(8 ** -0.5 * 64 ** -0.5)
NEG = -30000.0
NBIS = 22

DEBUG = False
DEBUG_FLUSH = False
STOP_AFTER_FLUSH = 0


class _Stop(Exception):
    pass


class T:
    __slots__ = ("w", "r", "excl")

    def __init__(self, excl=False):
        self.w = None
        self.r = {}
        self.excl = excl


def Ts(n, excl=False):
    return [T(excl) for _ in range(n)]


class FW:
    ENGS = ("tensor", "vector", "scalar", "gpsimd", "sync")
    NDMA = 8

    def __init__(self, nc, stack):
        self.nc = nc
        self.ops = {e: [] for e in self.ENGS}
        self.cnt = {e: 0 for e in self.ENGS}
        self.waited = {e: {} for e in self.ENGS}
        self.dma_i = 0
        self.final = {}
        keys = list(self.ENGS) + ["dma%d" % i for i in range(self.NDMA)]
        self.sems = {k: stack.enter_context(nc.semaphore("s_" + k)) for k in keys}
        self.nops = 0
        self.nflush = 0
        self.rank_base = {e: 0 for e in self.ENGS}
        self.flushed = {e: 0 for e in self.ENGS}

    def _need(self, eng, dep, waits):
        if dep is None:
            return
        k, v = dep
        if self.waited[eng].get(k, 0) >= v:
            return
        if k == eng and eng == "tensor":
            return
        waits[k] = max(waits.get(k, 0), v)

    def _deps(self, eng, reads, writes):
        waits = {}
        for t in reads:
            self._need(eng, t.w, waits)
            if t.excl:
                for k, v in t.r.items():
                    if k != eng:
                        self._need(eng, (k, v), waits)
        for t in writes:
            self._need(eng, t.w, waits)
            for k, v in t.r.items():
                self._need(eng, (k, v), waits)
        for k, v in waits.items():
            self.waited[eng][k] = v
        return waits

    def _mark(self, key, val, reads, writes):
        for t in reads:
            t.r[key] = max(t.r.get(key, 0), val)
        for t in writes:
            t.w = (key, val)
            t.r = {}
        self.final[key] = max(self.final.get(key, 0), val)

    def op(self, eng, name, kw, reads=(), writes=()):
        reads = tuple(reads)
        writes = tuple(writes)
        waits = self._deps(eng, reads, writes)
        self.cnt[eng] += 1
        self.ops[eng].append((tuple(waits.items()), name, kw, eng, self.cnt[eng]))
        self._mark(eng, self.cnt[eng], reads, writes)
        self.nops += 1

    def dma(self, out, in_, reads=(), writes=()):
        reads = tuple(reads)
        writes = tuple(writes)
        i = self.dma_i
        self.dma_i += 1
        key = "dma%d" % (i % self.NDMA)
        val = 16 * (i // self.NDMA + 1)
        waits = self._deps("sync", reads, writes)
        if val > 16 and self.waited["sync"].get(key, 0) < val - 16:
            waits[key] = val - 16
            self.waited["sync"][key] = val - 16
        self.cnt["sync"] += 1
        self.ops["sync"].append((tuple(waits.items()), "dma_start", dict(out=out, in_=in_), key, self.cnt["sync"]))
        self._mark(key, val, reads, writes)
        self.nops += 1

    def flush(self):
        nc = self.nc
        sems = self.sems
        ops = self.ops
        ms = {e: set() for e in self.ENGS}
        for e in self.ENGS:
            for waits, _n, _kw, _key, _idx in ops[e]:
                for k, v in waits:
                    if k in ms:
                        ms[k].add(v)
        for e in self.ENGS:
            if e != "sync" and self.cnt[e] > self.flushed[e]:
                ms[e].add(self.cnt[e])
        rank = {}
        for e in self.ENGS:
            for i, v in enumerate(sorted(ms[e])):
                rank[(e, v)] = self.rank_base[e] + i + 1
        final_waits = []
        for e in self.ENGS:
            if e != "sync" and self.cnt[e] > self.flushed[e]:
                final_waits.append((sems[e], rank[(e, self.cnt[e])]))
        for k, v in self.final.items():
            if k.startswith("dma"):
                final_waits.append((sems[k], v))

        def wval(k, v):
            return v if k.startswith("dma") else rank[(k, v)]

        with nc.Block() as block:
            def body(ename):
                def f(e):
                    for waits, name, kw, key, idx in ops[ename]:
                        for k, v in waits:
                            e.wait_ge(sems[k], wval(k, v))
                        ins = getattr(e, name)(**kw)
                        if key.startswith("dma"):
                            ins.then_inc(sems[key], 16)
                        elif (ename, idx) in rank:
                            ins.then_inc(sems[ename], 1)
                    if ename == "sync":
                        for s, v in final_waits:
                            e.wait_ge(s, v)
                return f
            block.tensor(body("tensor"))
            block.vector(body("vector"))
            block.scalar(body("scalar"))
            block.gpsimd(body("gpsimd"))
            block.sync(body("sync"))
        for e in self.ENGS:
            self.rank_base[e] += len(ms[e])
            self.flushed[e] = self.cnt[e]
        self.ops = {e: [] for e in self.ENGS}
        done = dict(self.final)
        for e in self.ENGS:
            done[e] = self.cnt[e]
        for e in self.ENGS:
            self.waited[e] = dict(done)
        self.nflush += 1
        if STOP_AFTER_FLUSH and self.nflush >= STOP_AFTER_FLUSH:
            raise _Stop()

    def clear_sems(self):
        nc = self.nc
        sems = self.sems
        with nc.Block() as block:
            def f(e):
                for s in sems.values():
                    e.sem_clear(s)
            block.sync(f)


def build_nc():
    nc = bass.Bass("TRN2", target_bir_lowering=False)

    def din(name, shape):
        return nc.dram_tensor(name, list(shape), F32, kind="ExternalInput").ap()

    x_d = din("x", [L, D])
    w_in = din("w_in", [D, INW])
    w_uq = din("w_uq", [256, 1024])
    w_uk = din("w_uk", [16, 64, 256])
    w_uv = din("w_uv", [16, 256, 64])
    w_iq = din("w_iq", [256, 512])
    w_ba = din("w_branch_a", [D, D])
    w_bb = din("w_branch_b", [D, D])
    w_out = din("w_out", [D, D])
    w_gate = din("w_gate", [D, DFF])
    w_up = din("w_up", [D, DFF])
    w_down = din("w_down", [DFF, D])
    mixg_d = din("mixg_col", [128, 8])
    ffng_d = din("ffng_col", [128, 8])
    fing_d = din("fing_row", [128, D])
    cqg_d = din("cqg_row", [128, 256])
    ckvg_d = din("ckvg_row", [128, 256])
    klng_d = din("klng_row", [128, 64])
    klnb_d = din("klnb_row", [128, 64])
    conv_d = din("conv_col", [128, 24, 4])
    alog_d = din("alog_row", [128, 8])
    dtb_d = din("dtb_row", [128, 8])
    ong_d = din("onormg_col", [128, 1])
    ident_d = din("ident", [128, 128])
    uincl_d = din("uincl", [128, 128])
    lstr_d = din("lstrict", [128, 128])
    out_d = nc.dram_tensor("out", [L, D], F32, kind="ExternalOutput").ap()
    skind = "ExternalOutput" if DEBUG else "Internal"
    x2_d = nc.dram_tensor("x2_scratch", [L, D], F32, kind=skind).ap()
    oa_d = nc.dram_tensor("oa_scratch", [8, 128, L], BF16, kind=skind).ap()
    ob_d = nc.dram_tensor("ob_scratch", [8, 128, L], BF16, kind=skind).ap()
    oa_v = oa_d.rearrange("h p t -> p h t")
    ob_v = ob_d.rearrange("h p t -> p h t")
    t_oad, t_obd, t_x2d = T(), T(), T()

    w_in_v = w_in.rearrange("(kc p) n -> p kc n", p=128)

    try:
      with ExitStack() as top:
        fw = FW(nc, top)
        fw.clear_sems()

        def sb(st, name, shape, dt):
            return st.enter_context(nc.sbuf_tensor(name, list(shape), dt))

        def pst(st, name, shape, dt):
            return st.enter_context(nc.psum_tensor(name, list(shape), dt))

        def V(name, r, w, **kw):
            fw.op("vector", name, kw, r, w)

        def A(name, r, w, **kw):
            fw.op("scalar", name, kw, r, w)

        def G(name, r, w, **kw):
            fw.op("gpsimd", name, kw, r, w)

        def P(name, r, w, **kw):
            fw.op("tensor", name, kw, r, w)

        def mm(out, lhsT, rhs, start, stop, r, w):
            fw.op("tensor", "matmul", dict(out=out, lhsT=lhsT, rhs=rhs, start=start, stop=stop), r, w)

        def rstd(t, src_, tmp_, dst_, scale, bias):
            A("activation", [t], [t], out=tmp_, in_=src_, func=AF.Ln, scale=scale, bias=bias)
            A("activation", [t], [t], out=dst_, in_=tmp_, func=AF.Exp, scale=-0.5)

        ident_f = sb(top, "ident_f", [128, 128], F32)
        ident_b = sb(top, "ident_b", [128, 128], BF16)
        ones_b = sb(top, "ones_b", [128, 128], BF16)
        ones_f = sb(top, "ones_f", [128, 128], F32)
        uincl_f = sb(top, "uincl_f", [128, 128], F32)
        lstr_f = sb(top, "lstr_f", [128, 128], F32)
        stg = [sb(top, "stg%d" % i, [128, 1024], F32) for i in range(3)]
        t_stg = Ts(3)
        stg_i = [0]
        t_const = T()
        mix = ExitStack()
        hT = sb(mix, "hT", [128, 8, L], BF16)
        t_hT = Ts(NT)
        beta = sb(mix, "beta", [128, NT, 8], F32)
        nbeta = sb(mix, "nbeta", [128, NT, 8], F32)
        gdec = sb(mix, "gdec", [128, NT, 8], F32)
        Gc = sb(mix, "Gc", [128, NT, 8], F32)
        bg = sb(mix, "bg", [128, NT, 8], F32)
        eTail = sb(mix, "eTail", [128, NT, 8], F32)
        aTail = sb(mix, "aTail", [128, NT, 8], F32)
        t_dn = T()
        att = ExitStack()
        cqT = sb(att, "cqT", [128, 2, L], BF16)
        ckvT = sb(att, "ckvT", [128, 2, L], BF16)
        ckv1 = sb(att, "ckv1", [128, NT, 256], BF16)
        kiT = sb(att, "kiT", [64, L], BF16)
        absw = sb(att, "absw", [128, NT, 8], F32)
        sgnw = sb(att, "sgnw", [128, NT, 8], F32)
        t_cqT, t_ckvT, t_ckv1, t_kiT = Ts(NT), Ts(NT), Ts(NT), Ts(NT)
        t_w = T()

        fw.dma(ident_f[:], ident_d, writes=[t_const])
        fw.dma(uincl_f[:], uincl_d, writes=[t_const])
        fw.dma(lstr_f[:], lstr_d, writes=[t_const])
        V("tensor_copy", [t_const], [t_const], out=ident_b[:], in_=ident_f[:])
        V("memset", [], [t_const], ap=ones_b[:], constant=1.0)
        V("memset", [], [t_const], ap=ones_f[:], constant=1.0)

        cast_engs = ["gpsimd"]

        def cast(dst, src_, r, w):
            e = cast_engs[stg_i[0] % len(cast_engs)]
            fw.op(e, "copy" if e == "scalar" else "tensor_copy", dict(out=dst, in_=src_), r, w)

        def wload(dst, src, t_dst, width):
            i = stg_i[0] % 3
            stg_i[0] += 1
            fw.dma(stg[i][:, 0:width], src, writes=[t_stg[i]])
            cast(dst, stg[i][:, 0:width], [t_stg[i]], [t_dst])

        def wload3(dst, src, t_dst, a, b):
            i = stg_i[0] % 3
            stg_i[0] += 1
            sv = stg[i][:, 0:a * b].rearrange("p (a b) -> p a b", a=a)
            fw.dma(sv, src, writes=[t_stg[i]])
            cast(dst, sv, [t_stg[i]], [t_dst])

        def rmsnorm_T(st_name, xt, t_xt, gB, dstT, t_dst, col0, res):
            junk, t_junk, ss, t_ss, xs, t_xs, pT, t_pT = res
            A("activation", [t_xt], [t_junk, t_ss], out=junk[:], in_=xt, func=AF.Square, accum_out=ss[:, 0:1])
            rstd(t_ss, ss[:, 0:1], ss[:, 1:2], ss[:, 3:4], 1.0 / D, EPS)
            V("tensor_scalar", [t_xt, t_ss], [t_xs], out=xs[:], in0=xt, scalar1=ss[:, 3:4], scalar2=None,
              op0=ALU.mult)
            for kc in range(8):
                P("transpose", [t_xs, t_const], [t_pT], out=pT[:, kc, :], in_=xs[:, kc * 128:(kc + 1) * 128],
                  identity=ident_b[:])
            V("tensor_tensor", [t_pT, t_const], [t_dst], out=dstT[:, :, col0:col0 + 128], in0=pT[:], in1=gB[:],
              op=ALU.mult)

        with ExitStack() as ph:
            gB = sb(ph, "gB", [128, 8, 128], F32)
            mixg = sb(ph, "mixg", [128, 8], F32)
            xin = [sb(ph, "xin%d" % i, [128, D], F32) for i in range(2)]
            t_xin = Ts(2)
            junk = sb(ph, "junk", [128, D], F32)
            ss = sb(ph, "ss", [128, 8], F32)
            xs = sb(ph, "xs", [128, D], BF16)
            pT = pst(ph, "pT", [128, 8, 128], BF16)
            res = (junk, T(), ss, T(), xs, T(), pT, T(True))
            wA = sb(ph, "wA", [128, 8, 584], BF16)
            wBA = sb(ph, "wBA", [128, 8, 16], BF16)
            t_wA = T()
            cqg = sb(ph, "cqg", [128, 256], F32)
            ckvg = sb(ph, "ckvg", [128, 256], F32)
            klng = sb(ph, "klng", [128, 64], F32)
            klnb = sb(ph, "klnb", [128, 64], F32)
            alog = sb(ph, "alog", [128, 8], F32)
            dtb = sb(ph, "dtb", [128, 8], F32)
            nalog = sb(ph, "nalog", [128, 8], F32)
            t_sm = T()
            psA = pst(ph, "psA", [128, 512], F32)
            psB = pst(ph, "psB", [128, 512], F32)
            psT2 = pst(ph, "psT2", [128, 8, 128], BF16)
            t_psA, t_psB, t_psT2 = T(True), T(True), T(True)
            st8 = sb(ph, "st8", [128, 16], F32)
            t_st8 = T()
            nrm = sb(ph, "nrm", [128, 512], BF16)
            t_nrm = T()
            kn = sb(ph, "kn", [128, 64], F32)
            knb = sb(ph, "knb", [128, 64], BF16)
            bst = sb(ph, "bst", [128, 8], F32)
            t_kn = T()
            sp = sb(ph, "sp", [128, 16], F32)
            t_sp = T()

            fw.dma(mixg[:], mixg_d, writes=[t_sm])
            for a_, b_ in ((cqg, cqg_d), (ckvg, ckvg_d), (klng, klng_d), (klnb, klnb_d), (alog, alog_d), (dtb, dtb_d)):
                fw.dma(a_[:], b_, writes=[t_sm])
            for kc in range(8):
                V("tensor_scalar", [t_const, t_sm], [t_const], out=gB[:, kc, :], in0=ones_f[:],
                  scalar1=mixg[:, kc:kc + 1], scalar2=None, op0=ALU.mult)
            A("activation", [t_sm], [t_sm], out=nalog[:], in_=alog[:], func=AF.Exp)
            V("tensor_scalar", [t_sm], [t_sm], out=nalog[:], in0=nalog[:], scalar1=-1.0, scalar2=None, op0=ALU.mult)
            for kc in range(8):
                wload(wA[:, kc, :], w_in_v[:, kc, 0:584], t_wA, 584)
                wload(wBA[:, kc, :], w_in_v[:, kc, C_BETA:C_BETA + 16], t_wA, 16)
            V("memset", [], [t_ckv1[0]], ap=absw[:], constant=0.0)

            for tt in range(NT):
                sl = slice(tt * 128, (tt + 1) * 128)
                xt = xin[tt % 2]
                fw.dma(xt[:], x_d[sl, :], writes=[t_xin[tt % 2]])
                rmsnorm_T("p1", xt[:], t_xin[tt % 2], gB, hT, t_hT[tt], tt * 128, res)
                for kc in range(8):
                    mm(psA[:], hT[:, kc, sl], wA[:, kc, 0:512], kc == 0, kc == 7, [t_hT[tt], t_wA], [t_psA])
                for kc in range(8):
                    mm(psB[:, 0:72], hT[:, kc, sl], wA[:, kc, 512:584], kc == 0, kc == 7, [t_hT[tt], t_wA], [t_psB])
                for kc in range(8):
                    mm(psB[:, 96:112], hT[:, kc, sl], wBA[:, kc, :], kc == 0, kc == 7, [t_hT[tt], t_wA], [t_psB])
                for j, gg in ((0, cqg), (1, ckvg)):
                    cs = slice(j * 256, (j + 1) * 256)
                    A("activation", [t_psA], [res[1], t_st8], out=junk[:, 0:256], in_=psA[:, cs], func=AF.Square,
                      accum_out=st8[:, 4 * j:4 * j + 1])
                    rstd(t_st8, st8[:, 4 * j:4 * j + 1], st8[:, 4 * j + 1:4 * j + 2], st8[:, 4 * j + 3:4 * j + 4],
                         1.0 / 256, EPS)
                    V("scalar_tensor_tensor", [t_psA, t_st8, t_sm], [t_nrm], out=nrm[:, cs], in0=psA[:, cs],
                      scalar=st8[:, 4 * j + 3:4 * j + 4], in1=gg[:], op0=ALU.mult, op1=ALU.mult)
                G("tensor_copy", [t_nrm], [t_ckv1[tt]], out=ckv1[:, tt, :], in_=nrm[:, 256:512])
                for j in range(4):
                    P("transpose", [t_nrm, t_const], [t_psT2], out=psT2[:, j, :], in_=nrm[:, j * 128:(j + 1) * 128],
                      identity=ident_b[:])
                A("copy", [t_psT2], [t_cqT[tt]], out=cqT[:, :, sl], in_=psT2[:, 0:2, :])
                A("copy", [t_psT2], [t_ckvT[tt]], out=ckvT[:, :, sl], in_=psT2[:, 2:4, :])
                V("bn_stats", [t_psB], [t_st8], out=st8[:, 8:14], in_=psB[:, 0:64])
                V("bn_aggr", [t_st8], [t_st8], out=st8[:, 14:16], in_=st8[:, 8:14])
                rstd(t_st8, st8[:, 15:16], st8[:, 9:10], st8[:, 10:11], 1.0, EPS)
                V("tensor_scalar", [t_psB, t_st8], [t_kn], out=kn[:], in0=psB[:, 0:64], scalar1=st8[:, 14:15],
                  scalar2=st8[:, 10:11], op0=ALU.subtract, op1=ALU.mult)
                V("tensor_tensor", [t_kn, t_sm], [t_kn], out=kn[:], in0=kn[:], in1=klng[:], op=ALU.mult)
                V("tensor_tensor", [t_kn, t_sm], [t_kn], out=knb[:], in0=kn[:], in1=klnb[:], op=ALU.add)
                P("transpose", [t_kn, t_const], [t_psT2], out=psT2[0:64, 4, :], in_=knb[:], identity=ident_b[:])
                A("copy", [t_psT2], [t_kiT[tt]], out=kiT[:, sl], in_=psT2[0:64, 4, :])
                V("tensor_scalar", [t_psB], [t_w], out=sgnw[:, tt, :], in0=psB[:, 64:72], scalar1=0.0, scalar2=2.0,
                  op0=ALU.is_ge, op1=ALU.mult)
                V("tensor_scalar", [t_w], [t_w], out=sgnw[:, tt, :], in0=sgnw[:, tt, :], scalar1=-1.0, scalar2=None,
                  op0=ALU.add)
                V("scalar_tensor_tensor", [t_psB, t_w], [t_w], out=absw[:, tt, :], in0=psB[:, 64:72], scalar=IDX_SCALE,
                  in1=sgnw[:, tt, :], op0=ALU.mult, op1=ALU.mult)
                A("activation", [t_psB], [t_sp], out=sp[:, 0:8], in_=psB[:, 96:104], func=AF.Exp, scale=-1.0)
                V("tensor_scalar", [t_sp], [t_sp], out=sp[:, 0:8], in0=sp[:, 0:8], scalar1=1.0, scalar2=None,
                  op0=ALU.add)
                V("reciprocal", [t_sp], [t_dn], out=beta[:, tt, :], in_=sp[:, 0:8])
                V("tensor_tensor", [t_psB, t_sm], [t_sp], out=sp[:, 8:16], in0=psB[:, 104:112], in1=dtb[:], op=ALU.add)
                A("activation", [t_sp], [t_sp], out=sp[:, 8:16], in_=sp[:, 8:16], func=AF.Exp)
                A("activation", [t_sp], [t_sp], out=sp[:, 8:16], in_=sp[:, 8:16], func=AF.Ln, bias=1.0)
                V("tensor_tensor", [t_sp, t_sm], [t_dn], out=gdec[:, tt, :], in0=sp[:, 8:16], in1=nalog[:], op=ALU.mult)

            gflat = gdec[:].rearrange("p a b -> p (a b)")
            Gflat = Gc[:].rearrange("p a b -> p (a b)")
            mm(psB[:, 0:128], uincl_f[:], gflat, True, True, [t_dn, t_const], [t_psB])
            V("tensor_copy", [t_psB], [t_dn], out=Gflat, in_=psB[:, 0:128])
            mm(psB[:, 0:128], ones_f[:], gflat, True, True, [t_dn, t_const], [t_psB])
            A("activation", [t_psB], [t_dn], out=aTail[:].rearrange("p a b -> p (a b)"), in_=psB[:, 0:128], func=AF.Exp)
            V("tensor_tensor", [t_psB, t_dn], [t_dn], out=eTail[:].rearrange("p a b -> p (a b)"), in0=psB[:, 0:128],
              in1=Gflat, op=ALU.subtract)
            A("activation", [t_dn], [t_dn], out=eTail[:].rearrange("p a b -> p (a b)"),
              in_=eTail[:].rearrange("p a b -> p (a b)"), func=AF.Exp)
            A("activation", [t_dn], [t_dn], out=bg[:].rearrange("p a b -> p (a b)"), in_=Gflat, func=AF.Exp)
            V("tensor_tensor", [t_dn], [t_dn], out=bg[:].rearrange("p a b -> p (a b)"),
              in0=bg[:].rearrange("p a b -> p (a b)"), in1=beta[:].rearrange("p a b -> p (a b)"), op=ALU.mult)
            V("tensor_scalar", [t_dn], [t_dn], out=nbeta[:].rearrange("p a b -> p (a b)"),
              in0=beta[:].rearrange("p a b -> p (a b)"), scalar1=-1.0, scalar2=None, op0=ALU.mult)
            fw.flush()

        with ExitStack() as ph:
            wh = [sb(ph, "wh%d" % m, [128, 8, 128], BF16) for m in range(4)]
            t_wh = Ts(4)
            convw = sb(ph, "convw", [128, 24, 4], F32)
            onormg = sb(ph, "onormg", [128, 1], F32)
            t_cw = T()
            xc = sb(ph, "xc", [128, 3 + L], F32)
            yc = sb(ph, "yc", [128, L], F32)
            sq = sb(ph, "sq", [128, L], BF16)
            qT = sb(ph, "qT", [128, L], BF16)
            kT = sb(ph, "kT", [128, L], BF16)
            vsT = sb(ph, "vsT", [128, L], BF16)
            zs = sb(ph, "zs", [128, L], BF16)
            obh = sb(ph, "obh", [128, L], BF16)
            t_xc, t_yc, t_sq, t_qT, t_kT, t_vsT, t_zs, t_obh = Ts(8)
            kbg = sb(ph, "kbg", [128, NT, 128], BF16)
            ktl = sb(ph, "ktl", [128, NT, 128], BF16)
            vb = sb(ph, "vb", [128, NT, 128], BF16)
            TTf = sb(ph, "TTf", [128, NT, 128], BF16)
            nwdT = sb(ph, "nwdT", [128, NT, 128], BF16)
            qkT = sb(ph, "qkT", [128, NT, 128], BF16)
            qdT = sb(ph, "qdT", [128, NT, 128], BF16)
            t_kbg, t_ktl, t_vb, t_TTf, t_nwdT, t_qkT, t_qdT = Ts(7)
            Pm = [sb(ph, "Pm%d" % i, [128, 8, 128], BF16) for i in range(2)]
            Qm = [sb(ph, "Qm%d" % i, [128, 8, 128], BF16) for i in range(2)]
            TTm = [sb(ph, "TTm%d" % i, [128, 8, 128], BF16) for i in range(2)]
            IPm = sb(ph, "IPm", [128, 8, 128], BF16)
            t_Pm, t_Qm, t_TTm = Ts(2), Ts(2), Ts(2)
            t_IPm = T()

            def f2(name):
                return [sb(ph, "%s%d" % (name, i), [128, 128], F32) for i in range(2)]
            gBn, Dm, dec, W1, DmT, decT, W2, eGb = [f2(nm) for nm in ("gBn", "Dm", "dec", "W1", "DmT", "decT", "W2", "eGb")]
            t_gBn, t_Dm, t_dec, t_W1, t_DmT, t_decT, t_W2, t_eGb = [Ts(2) for _ in range(8)]
            S = sb(ph, "S", [128, 128], F32)
            S_bf = sb(ph, "S_bf", [128, 128], BF16)
            u_bf = sb(ph, "u_bf", [128, 128], BF16)
            on_bf = sb(ph, "on_bf", [128, 128], BF16)
            jk = sb(ph, "jk", [128, 128], F32)
            st = sb(ph, "st", [128, 8], F32)
            t_S, t_Sbf, t_ubf, t_on, t_jk, t_st = Ts(6)
            B = [pst(ph, "B%d" % i, [128, 512], F32) for i in range(6)]
            H = [pst(ph, "H%d" % i, [128, 8, 128], BF16) for i in range(2)]
            t_B, t_H = Ts(6, True), Ts(2, True)
            ident8t = sb(ph, "ident8t", [128, 8, 128], F32)
            for j in range(8):
                G("tensor_copy", [t_const], [t_const], out=ident8t[:, j, :], in_=ident_f[:])
            ident4 = ident8t[:, 0:4, :]
            ident8 = ident8t[:]

            def b4(ap):
                return ap.rearrange("p (a b) -> p a b", a=4)

            bigL = sb(ph, "bigL", [128, 128], F32)
            nbigT = sb(ph, "nbigT", [128, 128], F32)
            V("tensor_scalar", [t_const], [t_const], out=bigL[:], in0=lstr_f[:], scalar1=30000.0, scalar2=30000.0,
              op0=ALU.mult, op1=ALU.subtract)
            V("tensor_scalar", [t_const], [t_const], out=bigL[:], in0=bigL[:], scalar1=-1.0, scalar2=None,
              op0=ALU.mult)
            V("tensor_scalar", [t_const], [t_const], out=nbigT[:], in0=lstr_f[:], scalar1=-30000.0, scalar2=None,
              op0=ALU.mult)
            fw.dma(convw[:], conv_d, writes=[t_cw])
            fw.dma(onormg[:], ong_d, writes=[t_cw])
            V("memset", [], [t_xc], ap=xc[:, 0:3], constant=0.0)

            for h in range(8):
                for m, base in enumerate((C_QB, C_KB, C_VB, C_Z)):
                    wload3(wh[m][:], w_in_v[:, :, base + h * 128:base + (h + 1) * 128], t_wh[m], 8, 128)
                for m in range(4):
                    for tb in range(4):
                        bk = (m * 4 + tb) % 2
                        tsl = slice(tb * 512, (tb + 1) * 512)
                        for kc in range(8):
                            mm(B[bk][:], wh[m][:, kc, :], hT[:, kc, tsl], kc == 0, kc == 7,
                               [t_wh[m]] + t_hT[tb * 4:(tb + 1) * 4], [t_B[bk]])
                        if m == 3:
                            A("activation", [t_B[bk]], [t_zs], out=zs[:, tsl], in_=B[bk][:], func=AF.Silu)
                        else:
                            A("copy", [t_B[bk]], [t_xc], out=xc[:, 3 + tb * 512:3 + (tb + 1) * 512], in_=B[bk][:])
                    if m == 3:
                        continue
                    cb = m * 8 + h
                    V("tensor_scalar", [t_xc, t_cw], [t_yc], out=yc[:], in0=xc[:, 0:L], scalar1=convw[:, cb, 0:1],
                      scalar2=None, op0=ALU.mult)
                    for j in range(1, 4):
                        V("scalar_tensor_tensor", [t_xc, t_cw, t_yc], [t_yc], out=yc[:], in0=xc[:, j:j + L],
                          scalar=convw[:, cb, j:j + 1], in1=yc[:], op0=ALU.mult, op1=ALU.add)
                    if m == 2:
                        A("activation", [t_yc], [t_vsT], out=vsT[:], in_=yc[:], func=AF.Silu)
                        continue
                    dst, t_dst = (qT, t_qT) if m == 0 else (kT, t_kT)
                    A("activation", [t_yc], [t_xc], out=xc[:, 3:3 + L], in_=yc[:], func=AF.Silu)
                    A("activation", [t_xc], [t_sq], out=sq[:], in_=xc[:, 3:3 + L], func=AF.Square)
                    sc_ = 128.0 if m == 0 else 1.0
                    for tb in range(4):
                        tsl = slice(tb * 512, (tb + 1) * 512)
                        mm(B[2][:], ones_b[:], sq[:, tsl], True, True, [t_const, t_sq], [t_B[2]])
                        A("activation", [t_B[2]], [t_yc], out=yc[:, tsl], in_=B[2][:], func=AF.Ln, scale=sc_,
                          bias=sc_ * EPS)
                    A("activation", [t_yc], [t_yc], out=yc[:], in_=yc[:], func=AF.Exp, scale=-0.5)
                    V("tensor_tensor", [t_xc, t_yc], [t_dst], out=dst[:], in0=xc[:, 3:3 + L], in1=yc[:], op=ALU.mult)
                if DEBUG_FLUSH and h == 0:
                    fw.flush()
                for g2 in range(2):
                    gs = slice(g2 * 8, (g2 + 1) * 8)
                    for j in range(8):
                        n = g2 * 8 + j
                        P("transpose", [t_kT, t_const], [t_H[0]], out=H[0][:, j, :], in_=kT[:, n * 128:(n + 1) * 128],
                          identity=ident_b[:])
                    V("tensor_tensor", [t_H[0], t_dn], [t_kbg], out=kbg[:, gs, :], in0=H[0][:],
                      in1=bg[:, gs, h].unsqueeze(2).to_broadcast([128, 8, 128]), op=ALU.mult)
                    V("tensor_tensor", [t_H[0], t_dn], [t_ktl], out=ktl[:, gs, :], in0=H[0][:],
                      in1=eTail[:, gs, h].unsqueeze(2).to_broadcast([128, 8, 128]), op=ALU.mult)
                    for j in range(8):
                        n = g2 * 8 + j
                        P("transpose", [t_vsT, t_const], [t_H[1]], out=H[1][:, j, :], in_=vsT[:, n * 128:(n + 1) * 128],
                          identity=ident_b[:])
                    V("tensor_tensor", [t_H[1], t_dn], [t_vb], out=vb[:, gs, :], in0=H[1][:],
                      in1=beta[:, gs, h].unsqueeze(2).to_broadcast([128, 8, 128]), op=ALU.mult)
                if DEBUG_FLUSH and h == 0:
                    fw.flush()
                for half in range(2):
                    for j in range(8):
                        n = half * 8 + j
                        i2 = n % 2
                        ns = slice(n * 128, (n + 1) * 128)
                        Gcol = Gc[:, n, h:h + 1]
                        V("tensor_scalar", [t_const, t_dn], [t_gBn[i2]], out=gBn[i2][:], in0=ones_f[:],
                          scalar1=gdec[:, n, h:h + 1], scalar2=None, op0=ALU.mult)
                        gb_ps = B[3 + i2][:, 0:128]
                        mm(gb_ps, gBn[i2][:], uincl_f[:], True, True, [t_gBn[i2], t_const], [t_B[3 + i2]])
                        V("scalar_tensor_tensor", [t_B[3 + i2], t_dn, t_const], [t_Dm[i2]], out=Dm[i2][:], in0=gb_ps,
                          scalar=Gcol, in1=bigL[:], op0=ALU.subtract, op1=ALU.max)
                        A("activation", [t_Dm[i2]], [t_W1[i2]], out=W1[i2][:], in_=Dm[i2][:], func=AF.Exp, scale=-1.0)
                        V("scalar_tensor_tensor", [t_B[3 + i2], t_dn, t_const], [t_DmT[i2]], out=DmT[i2][:], in0=gb_ps,
                          scalar=Gcol, in1=nbigT[:], op0=ALU.subtract, op1=ALU.min)
                        A("activation", [t_DmT[i2]], [t_W2[i2]], out=W2[i2][:], in_=DmT[i2][:], func=AF.Exp)
                        A("activation", [t_B[3 + i2]], [t_eGb[i2]], out=eGb[i2][:], in_=gb_ps, func=AF.Exp)
                        G("tensor_tensor", [t_qT, t_eGb[i2]], [t_qdT], out=qdT[:, n, :], in0=qT[:, ns], in1=eGb[i2][:],
                          op=ALU.mult)
                        mm(B[i2][:, 0:128], kT[:, ns], kT[:, ns], True, True, [t_kT], [t_B[i2]])
                        mm(B[i2][:, 128:256], kT[:, ns], qT[:, ns], True, True, [t_kT, t_qT], [t_B[i2]])
                        V("scalar_tensor_tensor", [t_B[i2], t_dn, t_W1[i2]], [t_Pm[0]], out=Pm[0][:, j, :],
                          in0=B[i2][:, 0:128], scalar=nbeta[:, n, h:h + 1], in1=W1[i2][:], op0=ALU.mult, op1=ALU.mult)
                        V("tensor_tensor", [t_B[i2], t_W2[i2]], [t_qkT], out=qkT[:, n, :], in0=B[i2][:, 128:256],
                          in1=W2[i2][:], op=ALU.mult)
                        P("transpose", [t_Pm[0], t_const], [t_H[1]], out=H[1][:, j, :], in_=Pm[0][:, j, :],
                          identity=ident_b[:])
                    if DEBUG_FLUSH and h == 0 and half == 0:
                        fw.flush()
                    A("copy", [t_H[1]], [t_Qm[0]], out=Qm[0][:], in_=H[1][:])
                    V("tensor_tensor", [t_H[1], t_const], [t_TTm[0]], out=TTm[0][:], in0=H[1][:], in1=ident8, op=ALU.add)
                    cur = 0
                    for k in range(6):
                        nxt = 1 - cur
                        for g4 in range(2):
                            cs = slice(g4 * 4, g4 * 4 + 4)
                            for j in range(4):
                                c = g4 * 4 + j
                                mm(B[2 + g4][:, j * 128:(j + 1) * 128], Qm[cur][:, c, :], Pm[cur][:, c, :], True, True,
                                   [t_Qm[cur], t_Pm[cur]], [t_B[2 + g4]])
                            A("copy", [t_B[2 + g4]], [t_Pm[nxt]], out=Pm[nxt][:, cs, :], in_=b4(B[2 + g4][:]))
                            V("tensor_tensor", [t_B[2 + g4], t_const], [t_IPm], out=IPm[:, cs, :], in0=b4(B[2 + g4][:]),
                              in1=ident4, op=ALU.add)
                            if k < 5:
                                for j in range(4):
                                    c = g4 * 4 + j
                                    mm(B[4 + g4][:, j * 128:(j + 1) * 128], Pm[cur][:, c, :], Qm[cur][:, c, :], True,
                                       True, [t_Qm[cur], t_Pm[cur]], [t_B[4 + g4]])
                                A("copy", [t_B[4 + g4]], [t_Qm[nxt]], out=Qm[nxt][:, cs, :], in_=b4(B[4 + g4][:]))
                            for j in range(4):
                                c = g4 * 4 + j
                                mm(B[g4][:, j * 128:(j + 1) * 128], IPm[:, c, :], TTm[cur][:, c, :], True, True,
                                   [t_IPm, t_TTm[cur]], [t_B[g4]])
                            if k == 5:
                                V("tensor_copy", [t_B[g4]], [t_TTf],
                                  out=TTf[:, half * 8 + g4 * 4:half * 8 + g4 * 4 + 4, :], in_=b4(B[g4][:]))
                            else:
                                V("tensor_copy", [t_B[g4]], [t_TTm[nxt]], out=TTm[nxt][:, cs, :], in_=b4(B[g4][:]))
                        cur = nxt
                        if DEBUG_FLUSH and h == 0 and half == 0:
                            fw.flush()
                    for g4 in range(2):
                        n0 = half * 8 + g4 * 4
                        for j in range(4):
                            mm(B[4 + g4][:, j * 128:(j + 1) * 128], kbg[:, n0 + j, :], TTf[:, n0 + j, :], True, True,
                               [t_kbg, t_TTf], [t_B[4 + g4]])
                        V("tensor_scalar", [t_B[4 + g4]], [t_nwdT], out=nwdT[:, n0:n0 + 4, :], in0=b4(B[4 + g4][:]),
                          scalar1=-1.0, scalar2=None, op0=ALU.mult)
                if DEBUG_FLUSH and h == 0:
                    fw.flush()
                for n in range(NT):
                    ns = slice(n * 128, (n + 1) * 128)
                    mm(B[0][:, 0:128], TTf[:, n, :], vb[:, n, :], True, n == 0, [t_TTf, t_vb], [t_B[0]])
                    if n > 0:
                        mm(B[0][:, 0:128], nwdT[:, n, :], S_bf[:], False, True, [t_nwdT, t_Sbf], [t_B[0]])
                    A("copy", [t_B[0]], [t_ubf], out=u_bf[:], in_=B[0][:, 0:128])
                    if n > 0:
                        mm(B[1][:, 0:128], qdT[:, n, :], S_bf[:], True, False, [t_qdT, t_Sbf], [t_B[1]])
                    mm(B[1][:, 0:128], qkT[:, n, :], u_bf[:], n == 0, True, [t_qkT, t_ubf], [t_B[1]])
                    if n < NT - 1:
                        mm(B[2][:, 0:128], ktl[:, n, :], u_bf[:], True, True, [t_ktl, t_ubf], [t_B[2]])
                        if n == 0:
                            V("tensor_copy", [t_B[2]], [t_Sbf], out=S_bf[:], in_=B[2][:, 0:128])
                            V("tensor_copy", [t_B[2]], [t_S], out=S[:], in_=B[2][:, 0:128])
                        else:
                            V("scalar_tensor_tensor", [t_S, t_dn, t_B[2]], [t_Sbf], out=S_bf[:], in0=S[:],
                              scalar=aTail[:, n, h:h + 1], in1=B[2][:, 0:128], op0=ALU.mult, op1=ALU.add)
                            V("scalar_tensor_tensor", [t_S, t_dn, t_B[2]], [t_S], out=S[:], in0=S[:],
                              scalar=aTail[:, n, h:h + 1], in1=B[2][:, 0:128], op0=ALU.mult, op1=ALU.add)
                    A("activation", [t_B[1]], [t_jk, t_st], out=jk[:], in_=B[1][:, 0:128], func=AF.Square,
                      accum_out=st[:, 0:1])
                    rstd(t_st, st[:, 0:1], st[:, 1:2], st[:, 3:4], 1.0 / 128, EPS)
                    V("tensor_scalar", [t_B[1], t_st], [t_on], out=on_bf[:], in0=B[1][:, 0:128], scalar1=st[:, 3:4],
                      scalar2=None, op0=ALU.mult)
                    P("transpose", [t_on, t_const], [t_H[0]], out=H[0][:, 0, :], in_=on_bf[:], identity=ident_b[:])
                    V("scalar_tensor_tensor", [t_H[0], t_cw, t_zs], [t_obh], out=obh[:, ns], in0=H[0][:, 0, :],
                      scalar=onormg[:, 0:1], in1=zs[:, ns], op0=ALU.mult, op1=ALU.mult)
                fw.dma(ob_d[h], obh[:], reads=[t_obh], writes=[t_obd])
                if DEBUG_FLUSH and h == 0:
                    fw.flush()
            fw.flush()

        with ExitStack() as ph:
            Mh = sb(ph, "Mh", [128, 2, 16, 256], BF16)
            wiq = sb(ph, "wiq", [128, 2, 512], BF16)
            wuvp = sb(ph, "wuvp", [128, 2, 16, 128], BF16)
            t_wt = T()
            F01 = [pst(ph, "F0", [128, 512], F32), pst(ph, "F1", [128, 512], F32)]
            FQ = pst(ph, "FQ", [128, 1024], F32)
            F45 = [pst(ph, "F4", [128, 512], F32), pst(ph, "F5", [128, 512], F32)]
            H0 = pst(ph, "AH0", [128, 8, 128], BF16)
            t_F01, t_FQ, t_F45 = Ts(2, True), Ts(2, True), Ts(2, True)
            t_H0 = T(True)
            with ExitStack() as ph2:
                wuq = sb(ph2, "wuq", [128, 2, 1024], BF16)
                wuqT = sb(ph2, "wuqT", [128, 8, 256], BF16)
                wuk = sb(ph2, "wuk", [128, 8, 256], BF16)
                for rc in range(2):
                    wload(wuq[:, rc, :], w_uq[rc * 128:(rc + 1) * 128, :], t_wt, 1024)
                    wload(wiq[:, rc, :], w_iq[rc * 128:(rc + 1) * 128, :], t_wt, 512)
                wukv = w_uk.rearrange("(hp two) d r -> (two d) hp r", two=2)
                for j in range(2):
                    wload3(wuk[:, j * 4:(j + 1) * 4, :], wukv[:, j * 4:(j + 1) * 4, :], t_wt, 4, 256)
                G("memset", [], [t_wt], ap=wuvp[:], constant=0.0)
                wuvv = w_uv.rearrange("h (rc r) d -> r rc h d", rc=2)
                for rc in range(2):
                    i = stg_i[0] % 3
                    stg_i[0] += 1
                    sv = stg[i][:, 0:1024].rearrange("p (h d) -> p h d", h=16)
                    fw.dma(sv, wuvv[:, rc, :, :], writes=[t_stg[i]])
                    for h in range(16):
                        G("tensor_copy", [t_stg[i]], [t_wt], out=wuvp[:, rc, h, (h % 2) * 64:(h % 2) * 64 + 64],
                          in_=sv[:, h, :])
                for rc in range(2):
                    for cb in range(8):
                        P("transpose", [t_wt, t_const], [t_H0], out=H0[:, cb, :],
                          in_=wuq[:, rc, cb * 128:(cb + 1) * 128], identity=ident_b[:])
                    V("tensor_copy", [t_H0], [t_wt], out=wuqT[:, :, rc * 128:(rc + 1) * 128], in_=H0[:])
                for h in range(16):
                    hp, two = h // 2, h % 2
                    ps_ = slice(two * 64, two * 64 + 64)
                    for rc in range(2):
                        mm(F01[rc][:, 0:256], wuqT[ps_, hp, rc * 128:(rc + 1) * 128], wuk[ps_, hp, :], True, True,
                           [t_wt], [t_F01[rc]])
                        A("copy", [t_F01[rc]], [t_wt], out=Mh[:, rc, h, :], in_=F01[rc][:, 0:256])
                fw.flush()

            sc = sb(ph, "sc", [128, L], F32)
            jnk = sb(ph, "jnk", [128, L], BF16)
            biasm = sb(ph, "biasm", [128, L], BF16)
            biasT = sb(ph, "biasT", [128, NT, 128], BF16)
            qlT = sb(ph, "qlT", [128, 2, 16, 128], BF16)
            qiT = sb(ph, "qiT", [64, 8, 128], BF16)
            rl = [sb(ph, "rl%d" % i, [128, 512], F32) for i in range(2)]
            pt = [sb(ph, "pt%d" % i, [128, 512], BF16) for i in range(2)]
            rden = sb(ph, "rden", [128, 512], F32)
            onT = sb(ph, "onT", [128, 2, 512], BF16)
            bis = sb(ph, "bis", [128, 8], F32)
            dl = sb(ph, "dl", [128, 32], F32)
            crow = sb(ph, "crow", [128, 32], F32)
            oaq = [sb(ph, "oaq%d" % i, [128, 8, 128], BF16) for i in range(2)]
            t_sc, t_jnk, t_biasm, t_biasT, t_qlT, t_qiT, t_rden, t_onT, t_bis = Ts(9)
            t_rl, t_pt, t_oaq = Ts(2), Ts(2), Ts(2)
            for k in range(32):
                V("memset", [], [t_bis], ap=crow[:, k:k + 1], constant=float(2.0 ** (-k)))

            AX = pst(ph, "AX", [128, 512], F32)
            t_AX = T(True)
            qlT2 = [qlT, sb(ph, "qlT_b", [128, 2, 16, 128], BF16)]
            biasT2 = [biasT, sb(ph, "biasT_b", [128, NT, 128], BF16)]
            t_qlT2, t_biasT2 = Ts(2), Ts(2)

            def stageA(qb):
                qs = slice(qb * 128, (qb + 1) * 128)
                Tk = (qb + 1) * 128
                qlT_, t_qlT_ = qlT2[qb % 2], t_qlT2[qb % 2]
                biasT_, t_biasT_ = biasT2[qb % 2], t_biasT2[qb % 2]
                for half in range(2):
                    for hh in range(4):
                        hi = half * 4 + hh
                        for rc in range(2):
                            mm(AX[0:64, hh * 128:(hh + 1) * 128], wiq[:, rc, hi * 64:(hi + 1) * 64], cqT[:, rc, qs],
                               rc == 0, rc == 1, [t_wt, t_cqT[qb]], [t_AX])
                    A("copy", [t_AX], [t_qiT], out=qiT[:, half * 4:(half + 1) * 4, :],
                      in_=AX[0:64, :].rearrange("p (a b) -> p a b", a=4))
                    yield
                for rco in range(2):
                    for hg in range(4):
                        for hl in range(4):
                            h = hg * 4 + hl
                            for rci in range(2):
                                mm(AX[:, hl * 128:(hl + 1) * 128], Mh[:, rci, h, rco * 128:(rco + 1) * 128],
                                   cqT[:, rci, qs], rci == 0, rci == 1, [t_wt, t_cqT[qb]], [t_AX])
                        yield
                        V("tensor_copy", [t_AX], [t_qlT_], out=qlT_[:, rco, hg * 4:(hg + 1) * 4, :],
                          in_=AX[:].rearrange("p (a b) -> p a b", a=4))
                for kg in range(qb // 4 + 1):
                    ncol = min(512, Tk - kg * 512)
                    ks = slice(kg * 512, kg * 512 + ncol)
                    kts = [t_kiT[j] for j in range(kg * 4, kg * 4 + ncol // 128)]
                    for hi in range(8):
                        b2 = hi % 2
                        mm(AX[:, 0:ncol], qiT[:, hi, :], kiT[:, ks], True, True, [t_qiT] + kts, [t_AX])
                        yield
                        A("activation", [t_AX, t_w], [t_rl[b2]], out=rl[b2][:, 0:ncol], in_=AX[:, 0:ncol],
                          func=AF.Relu, scale=absw[:, qb, hi:hi + 1])
                        if hi == 0:
                            V("tensor_scalar", [t_rl[b2], t_w], [t_sc], out=sc[:, ks], in0=rl[b2][:, 0:ncol],
                              scalar1=sgnw[:, qb, hi:hi + 1], scalar2=None, op0=ALU.mult)
                        else:
                            V("scalar_tensor_tensor", [t_rl[b2], t_w, t_sc], [t_sc], out=sc[:, ks],
                              in0=rl[b2][:, 0:ncol], scalar=sgnw[:, qb, hi:hi + 1], in1=sc[:, ks], op0=ALU.mult,
                              op1=ALU.add)
                if qb >= 2:
                    V("tensor_reduce", [t_sc], [t_bis], out=bis[:, 0:1], in_=sc[:, 0:Tk], axis=AX_.X, op=ALU.max,
                      apply_absolute_value=True)
                    V("tensor_scalar", [t_bis], [t_bis], out=dl[:], in0=crow[:], scalar1=bis[:, 0:1], scalar2=None,
                      op0=ALU.mult)
                    V("memset", [], [t_bis], ap=bis[:, 1:2], constant=0.0)
                G("affine_select", [t_sc], [t_sc], out=sc[:, qs], in_=sc[:, qs], pattern=[[-1, 128]],
                  compare_op=ALU.is_ge, fill=-1e30, base=0, channel_multiplier=1)
                yield
                if qb >= 2:
                    cur = 1
                    for k in range(NBIS):
                        nxt = 3 - cur
                        V("tensor_scalar", [t_sc, t_bis], [t_jnk, t_bis], out=jnk[:, 0:Tk], in0=sc[:, 0:Tk],
                          scalar1=bis[:, cur:cur + 1], scalar2=0.0, op0=ALU.is_ge, op1=ALU.add,
                          accum_out=bis[:, 3:4])
                        V("tensor_scalar", [t_bis], [t_bis], out=bis[:, 4:5], in0=bis[:, 3:4], scalar1=255.5,
                          scalar2=0.5, op0=ALU.is_gt, op1=ALU.subtract)
                        V("scalar_tensor_tensor", [t_bis], [t_bis], out=bis[:, nxt:nxt + 1], in0=bis[:, 4:5],
                          scalar=dl[:, k:k + 1], in1=bis[:, cur:cur + 1], op0=ALU.mult, op1=ALU.add)
                        cur = nxt
                        yield
                    V("tensor_tensor", [t_bis], [t_bis], out=bis[:, 5:6], in0=bis[:, cur:cur + 1],
                      in1=dl[:, NBIS:NBIS + 1], op=ALU.subtract)
                else:
                    V("memset", [], [t_bis], ap=bis[:, 5:6], constant=-1e29)
                V("tensor_scalar", [t_sc, t_bis], [t_biasm], out=biasm[:, 0:Tk], in0=sc[:, 0:Tk], scalar1=bis[:, 5:6],
                  scalar2=NEG, op0=ALU.is_lt, op1=ALU.mult)
                yield
                for kb0 in range(0, qb + 1, 8):
                    nk = min(8, qb + 1 - kb0)
                    for j in range(nk):
                        kb = kb0 + j
                        P("transpose", [t_biasm, t_const], [t_H0], out=H0[:, j, :],
                          in_=biasm[:, kb * 128:(kb + 1) * 128], identity=ident_b[:])
                    A("copy", [t_H0], [t_biasT_], out=biasT_[:, kb0:kb0 + nk, :], in_=H0[:, 0:nk, :])
                    yield

            def stageE(qb, filler):
                qs = slice(qb * 128, (qb + 1) * 128)
                qlT_, t_qlT_ = qlT2[qb % 2], t_qlT2[qb % 2]
                biasT_, t_biasT_ = biasT2[qb % 2], t_biasT2[qb % 2]
                psO = [FQ[:, 0:512], FQ[:, 512:1024]]
                psD = F45[0]
                psOA = F45[1]
                oq = oaq[qb % 2]
                t_oq = t_oaq[qb % 2]

                def fill(n):
                    for _ in range(n):
                        if filler is not None:
                            next(filler, None)

                def qk_scores(hg, kb):
                    b2 = kb % 2
                    kbs = slice(kb * 128, (kb + 1) * 128)
                    for rc in range(2):
                        mm(F01[b2][:], ckvT[:, rc, kbs],
                           qlT_[:, rc, hg * 4:(hg + 1) * 4, :].rearrange("p a b -> p (a b)"),
                           rc == 0, False, [t_ckvT[kb], t_qlT_], [t_F01[b2]])
                    mm(F01[b2][:], ident_b[:], biasT_[:, kb:kb + 1, :].to_broadcast([128, 4, 128]), False, True,
                       [t_const, t_biasT_], [t_F01[b2]])

                for hg in range(4):
                    qk_scores(hg, 0)
                    for kb in range(qb + 1):
                        b2 = kb % 2
                        if kb < qb:
                            qk_scores(hg, kb + 1)
                        A("activation", [t_F01[b2]], [t_pt[b2]], out=pt[b2][:], in_=F01[b2][:], func=AF.Exp,
                          scale=0.125)
                        for rc in range(2):
                            mm(psO[rc], ckv1[:, kb, rc * 128:(rc + 1) * 128], pt[b2][:], kb == 0, kb == qb,
                               [t_ckv1[kb], t_pt[b2]], [t_FQ[rc]])
                        mm(psD[:], ones_b[:], pt[b2][:], kb == 0, kb == qb, [t_const, t_pt[b2]], [t_F45[0]])
                        fill(2)
                    V("reciprocal", [t_F45[0]], [t_rden], out=rden[:], in_=psD[:])
                    for rc in range(2):
                        V("tensor_tensor", [t_FQ[rc], t_rden], [t_onT], out=onT[:, rc, :], in0=psO[rc], in1=rden[:],
                          op=ALU.mult)
                    for hpl in range(2):
                        hp = hg * 2 + hpl
                        k = 0
                        for two in range(2):
                            h = hp * 2 + two
                            hl = h - hg * 4
                            for rc in range(2):
                                mm(psOA[:, hpl * 128:(hpl + 1) * 128], wuvp[:, rc, h, :],
                                   onT[:, rc, hl * 128:(hl + 1) * 128], k == 0, k == 3, [t_wt, t_onT], [t_F45[1]])
                                k += 1
                    A("copy", [t_F45[1]], [t_oq], out=oq[:, hg * 2:hg * 2 + 2, :],
                      in_=psOA[:, 0:256].rearrange("p (a b) -> p a b", a=2))
                fw.dma(oa_v[:, :, qs], oq[:], reads=[t_oq], writes=[t_oad])

            for _ in stageA(0):
                pass
            for qb in range(NT):
                gen = stageA(qb + 1) if qb + 1 < NT else None
                stageE(qb, gen)
                if gen is not None:
                    for _ in gen:
                        pass
            fw.flush()
        att.close()

        with ExitStack() as ph:
            PA = sb(ph, "PA", [128, 8, D], BF16)
            PB = sb(ph, "PB", [128, 8, D], BF16)
            WO = sb(ph, "WO", [128, 8, D], BF16)
            t_PA, t_PB, t_WO = Ts(3)
            wga = sb(ph, "wga", [128, 8, D], BF16)
            wgb = sb(ph, "wgb", [128, 8, D], BF16)
            t_wga, t_wgb = T(), T()
            oab = sb(ph, "oab", [128, 8, 512], BF16)
            obb = sb(ph, "obb", [128, 8, 512], BF16)
            t_oab, t_obb = T(), T()
            sgA = sb(ph, "sgA", [128, 512], F32)
            sgB = sb(ph, "sgB", [128, 512], F32)
            tA = sb(ph, "tA", [128, 512], F32)
            tB = sb(ph, "tB", [128, 512], F32)
            t_sgA, t_sgB, t_tA, t_tB = Ts(4)
            mT = sb(ph, "mT", [128, 8, 512], BF16)
            t_mT = T()
            xt2 = [sb(ph, "xt2_%d" % i, [128, D], F32) for i in range(2)]
            x2t = [sb(ph, "x2t_%d" % i, [128, D], F32) for i in range(2)]
            t_xt2, t_x2t = Ts(2), Ts(2)
            B = [pst(ph, "MB%d" % i, [128, 512], F32) for i in range(6)]
            t_B = Ts(6, True)
            cast_engs[:] = ["scalar", "vector"]
            for kc in range(8):
                ks_ = slice(kc * 128, (kc + 1) * 128)
                wload(PA[:, kc, :], w_ba[ks_, :], t_PA, 1024)
                wload(PB[:, kc, :], w_bb[ks_, :], t_PB, 1024)
                wload(WO[:, kc, :], w_out[ks_, :], t_WO, 1024)
                wload(wga[:, kc, :], w_in_v[:, kc, C_GA:C_GA + D], t_wga, 1024)
                wload(wgb[:, kc, :], w_in_v[:, kc, C_GB:C_GB + D], t_wgb, 1024)
            cnt = 0
            for tb in range(4):
                tsl = slice(tb * 512, (tb + 1) * 512)
                fw.dma(oab[:], oa_v[:, :, tsl], reads=[t_oad], writes=[t_oab])
                fw.dma(obb[:], ob_v[:, :, tsl], reads=[t_obd], writes=[t_obb])
                for nc_ in range(8):
                    i2 = cnt % 2
                    cnt += 1
                    ncs = slice(nc_ * 128, (nc_ + 1) * 128)
                    hts = t_hT[tb * 4:(tb + 1) * 4]
                    for kc in range(8):
                        mm(B[0][:], wga[:, kc, ncs], hT[:, kc, tsl], kc == 0, kc == 7, [t_wga] + hts, [t_B[0]])
                    for kc in range(8):
                        mm(B[1][:], wgb[:, kc, ncs], hT[:, kc, tsl], kc == 0, kc == 7, [t_wgb] + hts, [t_B[1]])
                    A("activation", [t_B[0]], [t_sgA], out=sgA[:], in_=B[0][:], func=AF.Sigmoid)
                    A("activation", [t_B[1]], [t_sgB], out=sgB[:], in_=B[1][:], func=AF.Sigmoid)
                    for hp in range(8):
                        mm(B[2][:], PA[:, hp, ncs], oab[:, hp, :], hp == 0, hp == 7, [t_PA, t_oab], [t_B[2]])
                    for hh in range(8):
                        mm(B[3][:], PB[:, hh, ncs], obb[:, hh, :], hh == 0, hh == 7, [t_PB, t_obb], [t_B[3]])
                    V("tensor_tensor", [t_B[2], t_sgA], [t_tA], out=tA[:], in0=B[2][:], in1=sgA[:], op=ALU.mult)
                    V("tensor_tensor", [t_B[3], t_sgB], [t_tB], out=tB[:], in0=B[3][:], in1=sgB[:], op=ALU.mult)
                    G("tensor_tensor", [t_tA, t_tB], [t_mT], out=mT[:, nc_, :], in0=tA[:], in1=tB[:], op=ALU.add)
                for j in range(4):
                    tt = tb * 4 + j
                    i2 = tt % 2
                    rows = slice(tt * 128, (tt + 1) * 128)
                    fw.dma(xt2[i2][:], x_d[rows, :], writes=[t_xt2[i2]])
                    for half in range(2):
                        hs = slice(half * 512, (half + 1) * 512)
                        for nc_ in range(8):
                            mm(B[4 + half][:], mT[:, nc_, j * 128:(j + 1) * 128], WO[:, nc_, hs], nc_ == 0, nc_ == 7,
                               [t_mT, t_WO], [t_B[4 + half]])
                        V("tensor_tensor", [t_B[4 + half], t_xt2[i2]], [t_x2t[i2]], out=x2t[i2][:, hs],
                          in0=B[4 + half][:], in1=xt2[i2][:, hs], op=ALU.add)
                    fw.dma(x2_d[rows, :], x2t[i2][:], reads=[t_x2t[i2]], writes=[t_x2d])
            fw.flush()
        mix.close()

        with ExitStack() as ph:
            Wg = sb(ph, "Wg", [128, 8, DFF], BF16)
            Wu = sb(ph, "Wu", [128, 8, DFF], BF16)
            Wd = sb(ph, "Wd", [128, NFC, D], BF16)
            t_Wg, t_Wu, t_Wd = Ts(3)
            gBf = sb(ph, "gBf", [128, 8, 128], F32)
            ffng = sb(ph, "ffng", [128, 8], F32)
            fing = sb(ph, "fing", [128, D], F32)
            t_fg = T()
            x2t = [sb(ph, "fx2t_%d" % i, [128, D], F32) for i in range(2)]
            t_x2t = Ts(2)
            x3 = sb(ph, "x3", [128, D], F32)
            yo = sb(ph, "yo", [128, D], F32)
            xs = sb(ph, "fxs", [128, D], BF16)
            ss = sb(ph, "fss", [128, 8], F32)
            fs = sb(ph, "ffs", [128, 8], F32)
            h2T = sb(ph, "h2T", [128, 8, 256], BF16)
            act = sb(ph, "act", [128, NFC, 256], BF16)
            sg = [sb(ph, "sg%d" % i, [128, 256], F32) for i in range(2)]
            t_x3, t_yo, t_fs, t_h2T, t_act = Ts(5)
            t_sg = Ts(2)
            pT = pst(ph, "fpT", [128, 8, 128], BF16)
            B = [pst(ph, "FB%d" % i, [128, 512], F32) for i in range(6)]
            t_B = Ts(6, True)
            res = (yo, t_yo, ss, T(), xs, T(), pT, T(True))
            fw.dma(ffng[:], ffng_d, writes=[t_fg])
            fw.dma(fing[:], fing_d, writes=[t_fg])
            for kc in range(8):
                V("tensor_scalar", [t_const, t_fg], [t_fg], out=gBf[:, kc, :], in0=ones_f[:],
                  scalar1=ffng[:, kc:kc + 1], scalar2=None, op0=ALU.mult)
            for kc in range(8):
                ks_ = slice(kc * 128, (kc + 1) * 128)
                for c0 in (0, 1024, 2048):
                    w_ = min(1024, DFF - c0)
                    wload(Wg[:, kc, c0:c0 + w_], w_gate[ks_, c0:c0 + w_], t_Wg, w_)
                    wload(Wu[:, kc, c0:c0 + w_], w_up[ks_, c0:c0 + w_], t_Wu, w_)
            for fc in range(NFC):
                wload(Wd[:, fc, :], w_down[fc * 128:(fc + 1) * 128, :], t_Wd, 1024)
            for tb2 in range(8):
                for j in range(2):
                    tt = tb2 * 2 + j
                    fw.dma(x2t[j][:], x2_d[tt * 128:(tt + 1) * 128, :], reads=[t_x2d], writes=[t_x2t[j]])
                    rmsnorm_T("ffn", x2t[j][:], t_x2t[j], gBf, h2T, t_h2T, j * 128, res)
                for fc in range(NFC):
                    bk = fc % 2
                    fcs = slice(fc * 128, (fc + 1) * 128)
                    for kc in range(8):
                        mm(B[bk][:, 0:256], Wg[:, kc, fcs], h2T[:, kc, :], kc == 0, kc == 7, [t_Wg, t_h2T], [t_B[bk]])
                    for kc in range(8):
                        mm(B[bk][:, 256:512], Wu[:, kc, fcs], h2T[:, kc, :], kc == 0, kc == 7, [t_Wu, t_h2T],
                           [t_B[bk]])
                    A("activation", [t_B[bk]], [t_sg[bk]], out=sg[bk][:], in_=B[bk][:, 0:256], func=AF.Silu)
                    V("tensor_tensor", [t_B[bk], t_sg[bk]], [t_act], out=act[:, fc, :], in0=B[bk][:, 256:512],
                      in1=sg[bk][:], op=ALU.mult)
                for j in range(2):
                    tt = tb2 * 2 + j
                    for half in range(2):
                        bi = 2 + j * 2 + half
                        hs = slice(half * 512, (half + 1) * 512)
                        for fc in range(NFC):
                            mm(B[bi][:], act[:, fc, j * 128:(j + 1) * 128], Wd[:, fc, hs], fc == 0, fc == NFC - 1,
                               [t_act, t_Wd], [t_B[bi]])
                        V("tensor_tensor", [t_B[bi], t_x2t[j]], [t_x3], out=x3[:, hs], in0=B[bi][:],
                          in1=x2t[j][:, hs], op=ALU.add)
                    A("activation", [t_x3], [t_yo, t_fs], out=yo[:], in_=x3[:], func=AF.Square, accum_out=fs[:, 0:1])
                    rstd(t_fs, fs[:, 0:1], fs[:, 1:2], fs[:, 3:4], 1.0 / D, EPS)
                    V("scalar_tensor_tensor", [t_x3, t_fs, t_fg], [t_yo], out=yo[:], in0=x3[:], scalar=fs[:, 3:4],
                      in1=fing[:], op0=ALU.mult, op1=ALU.mult)
                    fw.dma(out_d[tt * 128:(tt + 1) * 128, :], yo[:], reads=[t_yo])
            fw.flush()
        print("bass ops recorded:", fw.nops)
    except _Stop:
        print("build truncated after flush", STOP_AFTER_FLUSH)
    return nc


_NC_CACHE = {}


def _rep(v, n=128):
    return np.ascontiguousarray(np.tile(np.asarray(v, np.float32).reshape(1, -1), (n, 1)))


def kernel(x, mix_norm_g, w_in, cq_norm_g, ckv_norm_g, w_uq, w_uk, w_uv, w_iq,
           kidx_ln_g, kidx_ln_b, w_branch_a, conv_w, a_log, dt_bias, onorm_g,
           w_branch_b, w_out, ffn_norm_g, w_gate, w_up, w_down, final_norm_g):
    f = lambda a: np.ascontiguousarray(np.asarray(a, dtype=np.float32))
    x = f(x)
    n = x.shape[0]
    if "nc" not in _NC_CACHE:
        _NC_CACHE["nc"] = build_nc()
    nc = _NC_CACHE["nc"]
    shared = {
        "w_in": f(w_in)[0], "w_uq": f(w_uq)[0], "w_uk": f(w_uk)[0], "w_uv": f(w_uv)[0], "w_iq": f(w_iq)[0],
        "w_branch_a": f(w_branch_a)[0], "w_branch_b": f(w_branch_b)[0], "w_out": f(w_out)[0],
        "w_gate": f(w_gate)[0], "w_up": f(w_up)[0], "w_down": f(w_down)[0],
        "mixg_col": np.ascontiguousarray(f(mix_norm_g)[0].reshape(8, 128).T),
        "ffng_col": np.ascontiguousarray(f(ffn_norm_g)[0].reshape(8, 128).T),
        "fing_row": _rep(final_norm_g),
        "cqg_row": _rep(f(cq_norm_g)[0]), "ckvg_row": _rep(f(ckv_norm_g)[0]),
        "klng_row": _rep(f(kidx_ln_g)[0]), "klnb_row": _rep(f(kidx_ln_b)[0]),
        "conv_col": np.ascontiguousarray(f(conv_w)[0].T.reshape(24, 128, 4).transpose(1, 0, 2)),
        "alog_row": _rep(f(a_log)[0]), "dtb_row": _rep(f(dt_bias)[0]),
        "onormg_col": np.ascontiguousarray(f(onorm_g)[0].reshape(128, 1)),
        "ident": np.eye(128, dtype=np.float32),
        "uincl": np.triu(np.ones((128, 128), np.float32)),
        "lstrict": np.tril(np.ones((128, 128), np.float32), -1),
    }
    in_maps = [dict(shared, x=x[i]) for i in range(n)]
    res = run_bass_kernel_spmd(nc, in_maps, core_ids=list(range(n)))
    out = np.stack([np.asarray(r["out"], dtype=np.float32) for r in res.results], axis=0)
    if DEBUG:
        kernel.debug = res.results
    return out
```

```python
from contextlib import ExitStack

import numpy as np
import concourse.bass as bass
import concourse.mybir as mybir
from concourse.bass_utils import run_bass_kernel_spmd

F32 = mybir.dt.float32
BF16 = mybir.dt.bfloat16
AF = mybir.ActivationFunctionType
ALU = mybir.AluOpType
AX_ = mybir.AxisListType

L = 2048
D = 1024
NT = 16
EPS = 1e-6
DFF = 2816
NFC = DFF // 128
INW = 6744
C_Q, C_KV, C_KI, C_WI = 0, 256, 512, 576
C_QB, C_KB, C_VB = 584, 1608, 2632
C_BETA, C_A = 3656, 3664
C_Z, C_GA, C_GB = 3672, 4696, 5720
IDX_SCALE = float(8 ** -0.5 * 64 ** -0.5)
NEG = -30000.0
NBIS = 22

DEBUG = False
DEBUG_FLUSH = False
STOP_AFTER_FLUSH = 0


class _Stop(Exception):
    pass


class T:
    __slots__ = ("w", "r", "excl")

    def __init__(self, excl=False):
        self.w = None
        self.r = {}
        self.excl = excl


def Ts(n, excl=False):
    return [T(excl) for _ in range(n)]


class FW:
    ENGS = ("tensor", "vector", "scalar", "gpsimd", "sync")
    NDMA = 8

    def __init__(self, nc, stack):
        self.nc = nc
        self.ops = {e: [] for e in self.ENGS}
        self.cnt = {e: 0 for e in self.ENGS}
        self.waited = {e: {} for e in self.ENGS}
        self.dma_i = 0
        self.final = {}
        keys = list(self.ENGS) + ["dma%d" % i for i in range(self.NDMA)]
        self.sems = {k: stack.enter_context(nc.semaphore("s_" + k)) for k in keys}
        self.nops = 0
        self.nflush = 0
        self.rank_base = {e: 0 for e in self.ENGS}
        self.flushed = {e: 0 for e in self.ENGS}

    def _need(self, eng, dep, waits):
        if dep is None:
            return
        k, v = dep
        if self.waited[eng].get(k, 0) >= v:
            return
        if k == eng and eng == "tensor":
            return
        waits[k] = max(waits.get(k, 0), v)

    def _deps(self, eng, reads, writes):
        waits = {}
        for t in reads:
            self._need(eng, t.w, waits)
            if t.excl:
                for k, v in t.r.items():
                    if k != eng:
                        self._need(eng, (k, v), waits)
        for t in writes:
            self._need(eng, t.w, waits)
            for k, v in t.r.items():
                self._need(eng, (k, v), waits)
        for k, v in waits.items():
            self.waited[eng][k] = v
        return waits

    def _mark(self, key, val, reads, writes):
        for t in reads:
            t.r[key] = max(t.r.get(key, 0), val)
        for t in writes:
            t.w = (key, val)
            t.r = {}
        self.final[key] = max(self.final.get(key, 0), val)

    def op(self, eng, name, kw, reads=(), writes=()):
        reads = tuple(reads)
        writes = tuple(writes)
        waits = self._deps(eng, reads, writes)
        self.cnt[eng] += 1
        self.ops[eng].append((tuple(waits.items()), name, kw, eng, self.cnt[eng]))
        self._mark(eng, self.cnt[eng], reads, writes)
        self.nops += 1

    def dma(self, out, in_, reads=(), writes=()):
        reads = tuple(reads)
        writes = tuple(writes)
        i = self.dma_i
        self.dma_i += 1
        key = "dma%d" % (i % self.NDMA)
        val = 16 * (i // self.NDMA + 1)
        waits = self._deps("sync", reads, writes)
        if val > 16 and self.waited["sync"].get(key, 0) < val - 16:
            waits[key] = val - 16
            self.waited["sync"][key] = val - 16
        self.cnt["sync"] += 1
        self.ops["sync"].append((tuple(waits.items()), "dma_start", dict(out=out, in_=in_), key, self.cnt["sync"]))
        self._mark(key, val, reads, writes)
        self.nops += 1

    def flush(self):
        nc = self.nc
        sems = self.sems
        ops = self.ops
        ms = {e: set() for e in self.ENGS}
        for e in self.ENGS:
            for waits, _n, _kw, _key, _idx in ops[e]:
                for k, v in waits:
                    if k in ms:
                        ms[k].add(v)
        for e in self.ENGS:
            if e != "sync" and self.cnt[e] > self.flushed[e]:
                ms[e].add(self.cnt[e])
        rank = {}
        for e in self.ENGS:
            for i, v in enumerate(sorted(ms[e])):
                rank[(e, v)] = self.rank_base[e] + i + 1
        final_waits = []
        for e in self.ENGS:
            if e != "sync" and self.cnt[e] > self.flushed[e]:
                final_waits.append((sems[e], rank[(e, self.cnt[e])]))
        for k, v in self.final.items():
            if k.startswith("dma"):
                final_waits.append((sems[k], v))

        def wval(k, v):
            return v if k.startswith("dma") else rank[(k, v)]

        with nc.Block() as block:
            def body(ename):
                def f(e):
                    for waits, name, kw, key, idx in ops[ename]:
                        for k, v in waits:
                            e.wait_ge(sems[k], wval(k, v))
                        ins = getattr(e, name)(**kw)
                        if key.startswith("dma"):
                            ins.then_inc(sems[key], 16)
                        elif (ename, idx) in rank:
                            ins.then_inc(sems[ename], 1)
                    if ename == "sync":
                        for s, v in final_waits:
                            e.wait_ge(s, v)
                return f
            block.tensor(body("tensor"))
            block.vector(body("vector"))
            block.scalar(body("scalar"))
            block.gpsimd(body("gpsimd"))
            block.sync(body("sync"))
        for e in self.ENGS:
            self.rank_base[e] += len(ms[e])
            self.flushed[e] = self.cnt[e]
        self.ops = {e: [] for e in self.ENGS}
        done = dict(self.final)
        for e in self.ENGS:
            done[e] = self.cnt[e]
        for e in self.ENGS:
            self.waited[e] = dict(done)
        self.nflush += 1
        if STOP_AFTER_FLUSH and self.nflush >= STOP_AFTER_FLUSH:
            raise _Stop()

    def clear_sems(self):
        nc = self.nc
        sems = self.sems
        with nc.Block() as block:
            def f(e):
                for s in sems.values():
                    e.sem_clear(s)
            block.sync(f)


def build_nc():
    nc = bass.Bass("TRN2", target_bir_lowering=False)

    def din(name, shape):
        return nc.dram_tensor(name, list(shape), F32, kind="ExternalInput").ap()

    x_d = din("x", [L, D])
    w_in = din("w_in", [D, INW])
    w_uq = din("w_uq", [256, 1024])
    w_uk = din("w_uk", [16, 64, 256])
    w_uv = din("w_uv", [16, 256, 64])
    w_iq = din("w_iq", [256, 512])
    w_ba = din("w_branch_a", [D, D])
    w_bb = din("w_branch_b", [D, D])
    w_out = din("w_out", [D, D])
    w_gate = din("w_gate", [D, DFF])
    w_up = din("w_up", [D, DFF])
    w_down = din("w_down", [DFF, D])
    mixg_d = din("mixg_col", [128, 8])
    ffng_d = din("ffng_col", [128, 8])
    fing_d = din("fing_row", [128, D])
    cqg_d = din("cqg_row", [128, 256])
    ckvg_d = din("ckvg_row", [128, 256])
    klng_d = din("klng_row", [128, 64])
    klnb_d = din("klnb_row", [128, 64])
    conv_d = din("conv_col", [128, 24, 4])
    alog_d = din("alog_row", [128, 8])
    dtb_d = din("dtb_row", [128, 8])
    ong_d = din("onormg_col", [128, 1])
    ident_d = din("ident", [128, 128])
    uincl_d = din("uincl", [128, 128])
    lstr_d = din("lstrict", [128, 128])
    out_d = nc.dram_tensor("out", [L, D], F32, kind="ExternalOutput").ap()
    skind = "ExternalOutput" if DEBUG else "Internal"
    x2_d = nc.dram_tensor("x2_scratch", [L, D], F32, kind=skind).ap()
    oa_d = nc.dram_tensor("oa_scratch", [8, 128, L], BF16, kind=skind).ap()
    ob_d = nc.dram_tensor("ob_scratch", [8, 128, L], BF16, kind=skind).ap()
    oa_v = oa_d.rearrange("h p t -> p h t")
    ob_v = ob_d.rearrange("h p t -> p h t")
    t_oad, t_obd, t_x2d = T(), T(), T()

    w_in_v = w_in.rearrange("(kc p) n -> p kc n", p=128)

    try:
      with ExitStack() as top:
        fw = FW(nc, top)
        fw.clear_sems()

        def sb(st, name, shape, dt):
            return st.enter_context(nc.sbuf_tensor(name, list(shape), dt))

        def pst(st, name, shape, dt):
            return st.enter_context(nc.psum_tensor(name, list(shape), dt))

        def V(name, r, w, **kw):
            fw.op("vector", name, kw, r, w)

        def A(name, r, w, **kw):
            fw.op("scalar", name, kw, r, w)

        def G(name, r, w, **kw):
            fw.op("gpsimd", name, kw, r, w)

        def P(name, r, w, **kw):
            fw.op("tensor", name, kw, r, w)

        def mm(out, lhsT, rhs, start, stop, r, w):
            fw.op("tensor", "matmul", dict(out=out, lhsT=lhsT, rhs=rhs, start=start, stop=stop), r, w)

        def rstd(t, src_, tmp_, dst_, scale, bias):
            A("activation", [t], [t], out=tmp_, in_=src_, func=AF.Ln, scale=scale, bias=bias)
            A("activation", [t], [t], out=dst_, in_=tmp_, func=AF.Exp, scale=-0.5)

        ident_f = sb(top, "ident_f", [128, 128], F32)
        ident_b = sb(top, "ident_b", [128, 128], BF16)
        ones_b = sb(top, "ones_b", [128, 128], BF16)
        ones_f = sb(top, "ones_f", [128, 128], F32)
        uincl_f = sb(top, "uincl_f", [128, 128], F32)
        lstr_f = sb(top, "lstr_f", [128, 128], F32)
        stg = [sb(top, "stg%d" % i, [128, 1024], F32) for i in range(3)]
        t_stg = Ts(3)
        stg_i = [0]
        t_const = T()
        mix = ExitStack()
        hT = sb(mix, "hT", [128, 8, L], BF16)
        t_hT = Ts(NT)
        beta = sb(mix, "beta", [128, NT, 8], F32)
        nbeta = sb(mix, "nbeta", [128, NT, 8], F32)
        gdec = sb(mix, "gdec", [128, NT, 8], F32)
        Gc = sb(mix, "Gc", [128, NT, 8], F32)
        bg = sb(mix, "bg", [128, NT, 8], F32)
        eTail = sb(mix, "eTail", [128, NT, 8], F32)
        aTail = sb(mix, "aTail", [128, NT, 8], F32)
        t_dn = T()
        att = ExitStack()
        cqT = sb(att, "cqT", [128, 2, L], BF16)
        ckvT = sb(att, "ckvT", [128, 2, L], BF16)
        ckv1 = sb(att, "ckv1", [128, NT, 256], BF16)
        kiT = sb(att, "kiT", [64, L], BF16)
        absw = sb(att, "absw", [128, NT, 8], F32)
        sgnw = sb(att, "sgnw", [128, NT, 8], F32)
        t_cqT, t_ckvT, t_ckv1, t_kiT = Ts(NT), Ts(NT), Ts(NT), Ts(NT)
        t_w = T()

        fw.dma(ident_f[:], ident_d, writes=[t_const])
        fw.dma(uincl_f[:], uincl_d, writes=[t_const])
        fw.dma(lstr_f[:], lstr_d, writes=[t_const])
        V("tensor_copy", [t_const], [t_const], out=ident_b[:], in_=ident_f[:])
        V("memset", [], [t_const], ap=ones_b[:], constant=1.0)
        V("memset", [], [t_const], ap=ones_f[:], constant=1.0)

        cast_engs = ["gpsimd"]

        def cast(dst, src_, r, w):
            e = cast_engs[stg_i[0] % len(cast_engs)]
            fw.op(e, "copy" if e == "scalar" else "tensor_copy", dict(out=dst, in_=src_), r, w)

        def wload(dst, src, t_dst, width):
            i = stg_i[0] % 3
            stg_i[0] += 1
            fw.dma(stg[i][:, 0:width], src, writes=[t_stg[i]])
            cast(dst, stg[i][:, 0:width], [t_stg[i]], [t_dst])

        def wload3(dst, src, t_dst, a, b):
            i = stg_i[0] % 3
            stg_i[0] += 1
            sv = stg[i][:, 0:a * b].rearrange("p (a b) -> p a b", a=a)
            fw.dma(sv, src, writes=[t_stg[i]])
            cast(dst, sv, [t_stg[i]], [t_dst])

        def rmsnorm_T(st_name, xt, t_xt, gB, dstT, t_dst, col0, res):
            junk, t_junk, ss, t_ss, xs, t_xs, pT, t_pT = res
            A("activation", [t_xt], [t_junk, t_ss], out=junk[:], in_=xt, func=AF.Square, accum_out=ss[:, 0:1])
            rstd(t_ss, ss[:, 0:1], ss[:, 1:2], ss[:, 3:4], 1.0 / D, EPS)
            V("tensor_scalar", [t_xt, t_ss], [t_xs], out=xs[:], in0=xt, scalar1=ss[:, 3:4], scalar2=None,
              op0=ALU.mult)
            for kc in range(8):
                P("transpose", [t_xs, t_const], [t_pT], out=pT[:, kc, :], in_=xs[:, kc * 128:(kc + 1) * 128],
                  identity=ident_b[:])
            V("tensor_tensor", [t_pT, t_const], [t_dst], out=dstT[:, :, col0:col0 + 128], in0=pT[:], in1=gB[:],
              op=ALU.mult)

        with ExitStack() as ph:
            gB = sb(ph, "gB", [128, 8, 128], F32)
            mixg = sb(ph, "mixg", [128, 8], F32)
            xin = [sb(ph, "xin%d" % i, [128, D], F32) for i in range(2)]
            t_xin = Ts(2)
            junk = sb(ph, "junk", [128, D], F32)
            ss = sb(ph, "ss", [128, 8], F32)
            xs = sb(ph, "xs", [128, D], BF16)
            pT = pst(ph, "pT", [128, 8, 128], BF16)
            res = (junk, T(), ss, T(), xs, T(), pT, T(True))
            wA = sb(ph, "wA", [128, 8, 584], BF16)
            wBA = sb(ph, "wBA", [128, 8, 16], BF16)
            t_wA = T()
            cqg = sb(ph, "cqg", [128, 256], F32)
            ckvg = sb(ph, "ckvg", [128, 256], F32)
            klng = sb(ph, "klng", [128, 64], F32)
            klnb = sb(ph, "klnb", [128, 64], F32)
            alog = sb(ph, "alog", [128, 8], F32)
            dtb = sb(ph, "dtb", [128, 8], F32)
            nalog = sb(ph, "nalog", [128, 8], F32)
            t_sm = T()
            psA = pst(ph, "psA", [128, 512], F32)
            psB = pst(ph, "psB", [128, 512], F32)
            psT2 = pst(ph, "psT2", [128, 8, 128], BF16)
            t_psA, t_psB, t_psT2 = T(True), T(True), T(True)
            st8 = sb(ph, "st8", [128, 16], F32)
            t_st8 = T()
            nrm = sb(ph, "nrm", [128, 512], BF16)
            t_nrm = T()
            kn = sb(ph, "kn", [128, 64], F32)
            knb = sb(ph, "knb", [128, 64], BF16)
            bst = sb(ph, "bst", [128, 8], F32)
            t_kn = T()
            sp = sb(ph, "sp", [128, 16], F32)
            t_sp = T()

            fw.dma(mixg[:], mixg_d, writes=[t_sm])
            for a_, b_ in ((cqg, cqg_d), (ckvg, ckvg_d), (klng, klng_d), (klnb, klnb_d), (alog, alog_d), (dtb, dtb_d)):
                fw.dma(a_[:], b_, writes=[t_sm])
            for kc in range(8):
                V("tensor_scalar", [t_const, t_sm], [t_const], out=gB[:, kc, :], in0=ones_f[:],
                  scalar1=mixg[:, kc:kc + 1], scalar2=None, op0=ALU.mult)
            A("activation", [t_sm], [t_sm], out=nalog[:], in_=alog[:], func=AF.Exp)
            V("tensor_scalar", [t_sm], [t_sm], out=nalog[:], in0=nalog[:], scalar1=-1.0, scalar2=None, op0=ALU.mult)
            for kc in range(8):
                wload(wA[:, kc, :], w_in_v[:, kc, 0:584], t_wA, 584)
                wload(wBA[:, kc, :], w_in_v[:, kc, C_BETA:C_BETA + 16], t_wA, 16)
            V("memset", [], [t_ckv1[0]], ap=absw[:], constant=0.0)

            for tt in range(NT):
                sl = slice(tt * 128, (tt + 1) * 128)
                xt = xin[tt % 2]
                fw.dma(xt[:], x_d[sl, :], writes=[t_xin[tt % 2]])
                rmsnorm_T("p1", xt[:], t_xin[tt % 2], gB, hT, t_hT[tt], tt * 128, res)
                for kc in range(8):
                    mm(psA[:], hT[:, kc, sl], wA[:, kc, 0:512], kc == 0, kc == 7, [t_hT[tt], t_wA], [t_psA])
                for kc in range(8):
                    mm(psB[:, 0:72], hT[:, kc, sl], wA[:, kc, 512:584], kc == 0, kc == 7, [t_hT[tt], t_wA], [t_psB])
                for kc in range(8):
                    mm(psB[:, 96:112], hT[:, kc, sl], wBA[:, kc, :], kc == 0, kc == 7, [t_hT[tt], t_wA], [t_psB])
                for j, gg in ((0, cqg), (1, ckvg)):
                    cs = slice(j * 256, (j + 1) * 256)
                    A("activation", [t_psA], [res[1], t_st8], out=junk[:, 0:256], in_=psA[:, cs], func=AF.Square,
                      accum_out=st8[:, 4 * j:4 * j + 1])
                    rstd(t_st8, st8[:, 4 * j:4 * j + 1], st8[:, 4 * j + 1:4 * j + 2], st8[:, 4 * j + 3:4 * j + 4],
                         1.0 / 256, EPS)
                    V("scalar_tensor_tensor", [t_psA, t_st8, t_sm], [t_nrm], out=nrm[:, cs], in0=psA[:, cs],
                      scalar=st8[:, 4 * j + 3:4 * j + 4], in1=gg[:], op0=ALU.mult, op1=ALU.mult)
                G("tensor_copy", [t_nrm], [t_ckv1[tt]], out=ckv1[:, tt, :], in_=nrm[:, 256:512])
                for j in range(4):
                    P("transpose", [t_nrm, t_const], [t_psT2], out=psT2[:, j, :], in_=nrm[:, j * 128:(j + 1) * 128],
                      identity=ident_b[:])
                A("copy", [t_psT2], [t_cqT[tt]], out=cqT[:, :, sl], in_=psT2[:, 0:2, :])
                A("copy", [t_psT2], [t_ckvT[tt]], out=ckvT[:, :, sl], in_=psT2[:, 2:4, :])
                V("bn_stats", [t_psB], [t_st8], out=st8[:, 8:14], in_=psB[:, 0:64])
                V("bn_aggr", [t_st8], [t_st8], out=st8[:, 14:16], in_=st8[:, 8:14])
                rstd(t_st8, st8[:, 15:16], st8[:, 9:10], st8[:, 10:11], 1.0, EPS)
                V("tensor_scalar", [t_psB, t_st8], [t_kn], out=kn[:], in0=psB[:, 0:64], scalar1=st8[:, 14:15],
                  scalar2=st8[:, 10:11], op0=ALU.subtract, op1=ALU.mult)
                V("tensor_tensor", [t_kn, t_sm], [t_kn], out=kn[:], in0=kn[:], in1=klng[:], op=ALU.mult)
                V("tensor_tensor", [t_kn, t_sm], [t_kn], out=knb[:], in0=kn[:], in1=klnb[:], op=ALU.add)
                P("transpose", [t_kn, t_const], [t_psT2], out=psT2[0:64, 4, :], in_=knb[:], identity=ident_b[:])
                A("copy", [t_psT2], [t_kiT[tt]], out=kiT[:, sl], in_=psT2[0:64, 4, :])
                V("tensor_scalar", [t_psB], [t_w], out=sgnw[:, tt, :], in0=psB[:, 64:72], scalar1=0.0, scalar2=2.0,
                  op0=ALU.is_ge, op1=ALU.mult)
                V("tensor_scalar", [t_w], [t_w], out=sgnw[:, tt, :], in0=sgnw[:, tt, :], scalar1=-1.0, scalar2=None,
                  op0=ALU.add)
                V("scalar_tensor_tensor", [t_psB, t_w], [t_w], out=absw[:, tt, :], in0=psB[:, 64:72], scalar=IDX_SCALE,
                  in1=sgnw[:, tt, :], op0=ALU.mult, op1=ALU.mult)
                A("activation", [t_psB], [t_sp], out=sp[:, 0:8], in_=psB[:, 96:104], func=AF.Exp, scale=-1.0)
                V("tensor_scalar", [t_sp], [t_sp], out=sp[:, 0:8], in0=sp[:, 0:8], scalar1=1.0, scalar2=None,
                  op0=ALU.add)
                V("reciprocal", [t_sp], [t_dn], out=beta[:, tt, :], in_=sp[:, 0:8])
                V("tensor_tensor", [t_psB, t_sm], [t_sp], out=sp[:, 8:16], in0=psB[:, 104:112], in1=dtb[:], op=ALU.add)
                A("activation", [t_sp], [t_sp], out=sp[:, 8:16], in_=sp[:, 8:16], func=AF.Exp)
                A("activation", [t_sp], [t_sp], out=sp[:, 8:16], in_=sp[:, 8:16], func=AF.Ln, bias=1.0)
                V("tensor_tensor", [t_sp, t_sm], [t_dn], out=gdec[:, tt, :], in0=sp[:, 8:16], in1=nalog[:], op=ALU.mult)

            gflat = gdec[:].rearrange("p a b -> p (a b)")
            Gflat = Gc[:].rearrange("p a b -> p (a b)")
            mm(psB[:, 0:128], uincl_f[:], gflat, True, True, [t_dn, t_const], [t_psB])
            V("tensor_copy", [t_psB], [t_dn], out=Gflat, in_=psB[:, 0:128])
            mm(psB[:, 0:128], ones_f[:], gflat, True, True, [t_dn, t_const], [t_psB])
            A("activation", [t_psB], [t_dn], out=aTail[:].rearrange("p a b -> p (a b)"), in_=psB[:, 0:128], func=AF.Exp)
            V("tensor_tensor", [t_psB, t_dn], [t_dn], out=eTail[:].rearrange("p a b -> p (a b)"), in0=psB[:, 0:128],
              in1=Gflat, op=ALU.subtract)
            A("activation", [t_dn], [t_dn], out=eTail[:].rearrange("p a b -> p (a b)"),
              in_=eTail[:].rearrange("p a b -> p (a b)"), func=AF.Exp)
            A("activation", [t_dn], [t_dn], out=bg[:].rearrange("p a b -> p (a b)"), in_=Gflat, func=AF.Exp)
            V("tensor_tensor", [t_dn], [t_dn], out=bg[:].rearrange("p a b -> p (a b)"),
              in0=bg[:].rearrange("p a b -> p (a b)"), in1=beta[:].rearrange("p a b -> p (a b)"), op=ALU.mult)
            V("tensor_scalar", [t_dn], [t_dn], out=nbeta[:].rearrange("p a b -> p (a b)"),
              in0=beta[:].rearrange("p a b -> p (a b)"), scalar1=-1.0, scalar2=None, op0=ALU.mult)
            fw.flush()

        with ExitStack() as ph:
            wh = [sb(ph, "wh%d" % m, [128, 8, 128], BF16) for m in range(4)]
            t_wh = Ts(4)
            convw = sb(ph, "convw", [128, 24, 4], F32)
            onormg = sb(ph, "onormg", [128, 1], F32)
            t_cw = T()
            xc = sb(ph, "xc", [128, 3 + L], F32)
            yc = sb(ph, "yc", [128, L], F32)
            sq = sb(ph, "sq", [128, L], BF16)
            qT = sb(ph, "qT", [128, L], BF16)
            kT = sb(ph, "kT", [128, L], BF16)
            vsT = sb(ph, "vsT", [128, L], BF16)
            zs = sb(ph, "zs", [128, L], BF16)
            obh = sb(ph, "obh", [128, L], BF16)
            t_xc, t_yc, t_sq, t_qT, t_kT, t_vsT, t_zs, t_obh = Ts(8)
            kbg = sb(ph, "kbg", [128, NT, 128], BF16)
            ktl = sb(ph, "ktl", [128, NT, 128], BF16)
            vb = sb(ph, "vb", [128, NT, 128], BF16)
            TTf = sb(ph, "TTf", [128, NT, 128], BF16)
            nwdT = sb(ph, "nwdT", [128, NT, 128], BF16)
            qkT = sb(ph, "qkT", [128, NT, 128], BF16)
            qdT = sb(ph, "qdT", [128, NT, 128], BF16)
            t_kbg, t_ktl, t_vb, t_TTf, t_nwdT, t_qkT, t_qdT = Ts(7)
            Pm = [sb(ph, "Pm%d" % i, [128, 8, 128], BF16) for i in range(2)]
            Qm = [sb(ph, "Qm%d" % i, [128, 8, 128], BF16) for i in range(2)]
            TTm = [sb(ph, "TTm%d" % i, [128, 8, 128], BF16) for i in range(2)]
            IPm = sb(ph, "IPm", [128, 8, 128], BF16)
            t_Pm, t_Qm, t_TTm = Ts(2), Ts(2), Ts(2)
            t_IPm = T()

            def f2(name):
                return [sb(ph, "%s%d" % (name, i), [128, 128], F32) for i in range(2)]
            gBn, Dm, dec, W1, DmT, decT, W2, eGb = [f2(nm) for nm in ("gBn", "Dm", "dec", "W1", "DmT", "decT", "W2", "eGb")]
            t_gBn, t_Dm, t_dec, t_W1, t_DmT, t_decT, t_W2, t_eGb = [Ts(2) for _ in range(8)]
            S = sb(ph, "S", [128, 128], F32)
            S_bf = sb(ph, "S_bf", [128, 128], BF16)
            u_bf = sb(ph, "u_bf", [128, 128], BF16)
            on_bf = sb(ph, "on_bf", [128, 128], BF16)
            jk = sb(ph, "jk", [128, 128], F32)
            st = sb(ph, "st", [128, 8], F32)
            t_S, t_Sbf, t_ubf, t_on, t_jk, t_st = Ts(6)
            B = [pst(ph, "B%d" % i, [128, 512], F32) for i in range(6)]
            H = [pst(ph, "H%d" % i, [128, 8, 128], BF16) for i in range(2)]
            t_B, t_H = Ts(6, True), Ts(2, True)
            ident8t = sb(ph, "ident8t", [128, 8, 128], F32)
            for j in range(8):
                G("tensor_copy", [t_const], [t_const], out=ident8t[:, j, :], in_=ident_f[:])
            ident4 = ident8t[:, 0:4, :]
            ident8 = ident8t[:]

            def b4(ap):
                return ap.rearrange("p (a b) -> p a b", a=4)

            bigL = sb(ph, "bigL", [128, 128], F32)
            nbigT = sb(ph, "nbigT", [128, 128], F32)
            V("tensor_scalar", [t_const], [t_const], out=bigL[:], in0=lstr_f[:], scalar1=30000.0, scalar2=30000.0,
              op0=ALU.mult, op1=ALU.subtract)
            V("tensor_scalar", [t_const], [t_const], out=bigL[:], in0=bigL[:], scalar1=-1.0, scalar2=None,
              op0=ALU.mult)
            V("tensor_scalar", [t_const], [t_const], out=nbigT[:], in0=lstr_f[:], scalar1=-30000.0, scalar2=None,
              op0=ALU.mult)
            fw.dma(convw[:], conv_d, writes=[t_cw])
            fw.dma(onormg[:], ong_d, writes=[t_cw])
            V("memset", [], [t_xc], ap=xc[:, 0:3], constant=0.0)

            for h in range(8):
                for m, base in enumerate((C_QB, C_KB, C_VB, C_Z)):
                    wload3(wh[m][:], w_in_v[:, :, base + h * 128:base + (h + 1) * 128], t_wh[m], 8, 128)
                for m in range(4):
                    for tb in range(4):
                        bk = (m * 4 + tb) % 2
                        tsl = slice(tb * 512, (tb + 1) * 512)
                        for kc in range(8):
                            mm(B[bk][:], wh[m][:, kc, :], hT[:, kc, tsl], kc == 0, kc == 7,
                               [t_wh[m]] + t_hT[tb * 4:(tb + 1) * 4], [t_B[bk]])
                        if m == 3:
                            A("activation", [t_B[bk]], [t_zs], out=zs[:, tsl], in_=B[bk][:], func=AF.Silu)
                        else:
                            A("copy", [t_B[bk]], [t_xc], out=xc[:, 3 + tb * 512:3 + (tb + 1) * 512], in_=B[bk][:])
                    if m == 3:
                        continue
                    cb = m * 8 + h
                    V("tensor_scalar", [t_xc, t_cw], [t_yc], out=yc[:], in0=xc[:, 0:L], scalar1=convw[:, cb, 0:1],
                      scalar2=None, op0=ALU.mult)
                    for j in range(1, 4):
                        V("scalar_tensor_tensor", [t_xc, t_cw, t_yc], [t_yc], out=yc[:], in0=xc[:, j:j + L],
                          scalar=convw[:, cb, j:j + 1], in1=yc[:], op0=ALU.mult, op1=ALU.add)
                    if m == 2:
                        A("activation", [t_yc], [t_vsT], out=vsT[:], in_=yc[:], func=AF.Silu)
                        continue
                    dst, t_dst = (qT, t_qT) if m == 0 else (kT, t_kT)
                    A("activation", [t_yc], [t_xc], out=xc[:, 3:3 + L], in_=yc[:], func=AF.Silu)
                    A("activation", [t_xc], [t_sq], out=sq[:], in_=xc[:, 3:3 + L], func=AF.Square)
                    sc_ = 128.0 if m == 0 else 1.0
                    for tb in range(4):
                        tsl = slice(tb * 512, (tb + 1) * 512)
                        mm(B[2][:], ones_b[:], sq[:, tsl], True, True, [t_const, t_sq], [t_B[2]])
                        A("activation", [t_B[2]], [t_yc], out=yc[:, tsl], in_=B[2][:], func=AF.Ln, scale=sc_,
                          bias=sc_ * EPS)
                    A("activation", [t_yc], [t_yc], out=yc[:], in_=yc[:], func=AF.Exp, scale=-0.5)
                    V("tensor_tensor", [t_xc, t_yc], [t_dst], out=dst[:], in0=xc[:, 3:3 + L], in1=yc[:], op=ALU.mult)
                if DEBUG_FLUSH and h == 0:
                    fw.flush()
                for g2 in range(2):
                    gs = slice(g2 * 8, (g2 + 1) * 8)
                    for j in range(8):
                        n = g2 * 8 + j
                        P("transpose", [t_kT, t_const], [t_H[0]], out=H[0][:, j, :], in_=kT[:, n * 128:(n + 1) * 128],
                          identity=ident_b[:])
                    V("tensor_tensor", [t_H[0], t_dn], [t_kbg], out=kbg[:, gs, :], in0=H[0][:],
                      in1=bg[:, gs, h].unsqueeze(2).to_broadcast([128, 8, 128]), op=ALU.mult)
                    V("tensor_tensor", [t_H[0], t_dn], [t_ktl], out=ktl[:, gs, :], in0=H[0][:],
                      in1=eTail[:, gs, h].unsqueeze(2).to_broadcast([128, 8, 128]), op=ALU.mult)
                    for j in range(8):
                        n = g2 * 8 + j
                        P("transpose", [t_vsT, t_const], [t_H[1]], out=H[1][:, j, :], in_=vsT[:, n * 128:(n + 1) * 128],
                          identity=ident_b[:])
                    V("tensor_tensor", [t_H[1], t_dn], [t_vb], out=vb[:, gs, :], in0=H[1][:],
                      in1=beta[:, gs, h].unsqueeze(2).to_broadcast([128, 8, 128]), op=ALU.mult)
                if DEBUG_FLUSH and h == 0:
                    fw.flush()
                for half in range(2):
                    for j in range(8):
                        n = half * 8 + j
                        i2 = n % 2
                        ns = slice(n * 128, (n + 1) * 128)
                        Gcol = Gc[:, n, h:h + 1]
                        V("tensor_scalar", [t_const, t_dn], [t_gBn[i2]], out=gBn[i2][:], in0=ones_f[:],
                          scalar1=gdec[:, n, h:h + 1], scalar2=None, op0=ALU.mult)
                        gb_ps = B[3 + i2][:, 0:128]
                        mm(gb_ps, gBn[i2][:], uincl_f[:], True, True, [t_gBn[i2], t_const], [t_B[3 + i2]])
                        V("scalar_tensor_tensor", [t_B[3 + i2], t_dn, t_const], [t_Dm[i2]], out=Dm[i2][:], in0=gb_ps,
                          scalar=Gcol, in1=bigL[:], op0=ALU.subtract, op1=ALU.max)
                        A("activation", [t_Dm[i2]], [t_W1[i2]], out=W1[i2][:], in_=Dm[i2][:], func=AF.Exp, scale=-1.0)
                        V("scalar_tensor_tensor", [t_B[3 + i2], t_dn, t_const], [t_DmT[i2]], out=DmT[i2][:], in0=gb_ps,
                          scalar=Gcol, in1=nbigT[:], op0=ALU.subtract, op1=ALU.min)
                        A("activation", [t_DmT[i2]], [t_W2[i2]], out=W2[i2][:], in_=DmT[i2][:], func=AF.Exp)
                        A("activation", [t_B[3 + i2]], [t_eGb[i2]], out=eGb[i2][:], in_=gb_ps, func=AF.Exp)
                        G("tensor_tensor", [t_qT, t_eGb[i2]], [t_qdT], out=qdT[:, n, :], in0=qT[:, ns], in1=eGb[i2][:],
                          op=ALU.mult)
                        mm(B[i2][:, 0:128], kT[:, ns], kT[:, ns], True, True, [t_kT], [t_B[i2]])
                        mm(B[i2][:, 128:256], kT[:, ns], qT[:, ns], True, True, [t_kT, t_qT], [t_B[i2]])
                        V("scalar_tensor_tensor", [t_B[i2], t_dn, t_W1[i2]], [t_Pm[0]], out=Pm[0][:, j, :],
                          in0=B[i2][:, 0:128], scalar=nbeta[:, n, h:h + 1], in1=W1[i2][:], op0=ALU.mult, op1=ALU.mult)
                        V("tensor_tensor", [t_B[i2], t_W2[i2]], [t_qkT], out=qkT[:, n, :], in0=B[i2][:, 128:256],
                          in1=W2[i2][:], op=ALU.mult)
                        P("transpose", [t_Pm[0], t_const], [t_H[1]], out=H[1][:, j, :], in_=Pm[0][:, j, :],
                          identity=ident_b[:])
                    if DEBUG_FLUSH and h == 0 and half == 0:
                        fw.flush()
                    A("copy", [t_H[1]], [t_Qm[0]], out=Qm[0][:], in_=H[1][:])
                    V("tensor_tensor", [t_H[1], t_const], [t_TTm[0]], out=TTm[0][:], in0=H[1][:], in1=ident8, op=ALU.add)
                    cur = 0
                    for k in range(6):
                        nxt = 1 - cur
                        for g4 in range(2):
                            cs = slice(g4 * 4, g4 * 4 + 4)
                            for j in range(4):
                                c = g4 * 4 + j
                                mm(B[2 + g4][:, j * 128:(j + 1) * 128], Qm[cur][:, c, :], Pm[cur][:, c, :], True, True,
                                   [t_Qm[cur], t_Pm[cur]], [t_B[2 + g4]])
                            A("copy", [t_B[2 + g4]], [t_Pm[nxt]], out=Pm[nxt][:, cs, :], in_=b4(B[2 + g4][:]))
                            V("tensor_tensor", [t_B[2 + g4], t_const], [t_IPm], out=IPm[:, cs, :], in0=b4(B[2 + g4][:]),
                              in1=ident4, op=ALU.add)
                            if k < 5:
                                for j in range(4):
                                    c = g4 * 4 + j
                                    mm(B[4 + g4][:, j * 128:(j + 1) * 128], Pm[cur][:, c, :], Qm[cur][:, c, :], True,
                                       True, [t_Qm[cur], t_Pm[cur]], [t_B[4 + g4]])
                                A("copy", [t_B[4 + g4]], [t_Qm[nxt]], out=Qm[nxt][:, cs, :], in_=b4(B[4 + g4][:]))
                            for j in range(4):
                                c = g4 * 4 + j
                                mm(B[g4][:, j * 128:(j + 1) * 128], IPm[:, c, :], TTm[cur][:, c, :], True, True,
                                   [t_IPm, t_TTm[cur]], [t_B[g4]])
                            if k == 5:
                                V("tensor_copy", [t_B[g4]], [t_TTf],
                                  out=TTf[:, half * 8 + g4 * 4:half * 8 + g4 * 4 + 4, :], in_=b4(B[g4][:]))
                            else:
                                V("tensor_copy", [t_B[g4]], [t_TTm[nxt]], out=TTm[nxt][:, cs, :], in_=b4(B[g4][:]))
                        cur = nxt
                        if DEBUG_FLUSH and h == 0 and half == 0:
                            fw.flush()
                    for g4 in range(2):
                        n0 = half * 8 + g4 * 4
                        for j in range(4):
                            mm(B[4 + g4][:, j * 128:(j + 1) * 128], kbg[:, n0 + j, :], TTf[:, n0 + j, :], True, True,
                               [t_kbg, t_TTf], [t_B[4 + g4]])
                        V("tensor_scalar", [t_B[4 + g4]], [t_nwdT], out=nwdT[:, n0:n0 + 4, :], in0=b4(B[4 + g4][:]),
                          scalar1=-1.0, scalar2=None, op0=ALU.mult)
                if DEBUG_FLUSH and h == 0:
                    fw.flush()
                for n in range(NT):
                    ns = slice(n * 128, (n + 1) * 128)
                    mm(B[0][:, 0:128], TTf[:, n, :], vb[:, n, :], True, n == 0, [t_TTf, t_vb], [t_B[0]])
                    if n > 0:
                        mm(B[0][:, 0:128], nwdT[:, n, :], S_bf[:], False, True, [t_nwdT, t_Sbf], [t_B[0]])
                    A("copy", [t_B[0]], [t_ubf], out=u_bf[:], in_=B[0][:, 0:128])
                    if n > 0:
                        mm(B[1][:, 0:128], qdT[:, n, :], S_bf[:], True, False, [t_qdT, t_Sbf], [t_B[1]])
                    mm(B[1][:, 0:128], qkT[:, n, :], u_bf[:], n == 0, True, [t_qkT, t_ubf], [t_B[1]])
                    if n < NT - 1:
                        mm(B[2][:, 0:128], ktl[:, n, :], u_bf[:], True, True, [t_ktl, t_ubf], [t_B[2]])
                        if n == 0:
                            V("tensor_copy", [t_B[2]], [t_Sbf], out=S_bf[:], in_=B[2][:, 0:128])
                            V("tensor_copy", [t_B[2]], [t_S], out=S[:], in_=B[2][:, 0:128])
                        else:
                            V("scalar_tensor_tensor", [t_S, t_dn, t_B[2]], [t_Sbf], out=S_bf[:], in0=S[:],
                              scalar=aTail[:, n, h:h + 1], in1=B[2][:, 0:128], op0=ALU.mult, op1=ALU.add)
                            V("scalar_tensor_tensor", [t_S, t_dn, t_B[2]], [t_S], out=S[:], in0=S[:],
                              scalar=aTail[:, n, h:h + 1], in1=B[2][:, 0:128], op0=ALU.mult, op1=ALU.add)
                    A("activation", [t_B[1]], [t_jk, t_st], out=jk[:], in_=B[1][:, 0:128], func=AF.Square,
                      accum_out=st[:, 0:1])
                    rstd(t_st, st[:, 0:1], st[:, 1:2], st[:, 3:4], 1.0 / 128, EPS)
                    V("tensor_scalar", [t_B[1], t_st], [t_on], out=on_bf[:], in0=B[1][:, 0:128], scalar1=st[:, 3:4],
                      scalar2=None, op0=ALU.mult)
                    P("transpose", [t_on, t_const], [t_H[0]], out=H[0][:, 0, :], in_=on_bf[:], identity=ident_b[:])
                    V("scalar_tensor_tensor", [t_H[0], t_cw, t_zs], [t_obh], out=obh[:, ns], in0=H[0][:, 0, :],
                      scalar=onormg[:, 0:1], in1=zs[:, ns], op0=ALU.mult, op1=ALU.mult)
                fw.dma(ob_d[h], obh[:], reads=[t_obh], writes=[t_obd])
                if DEBUG_FLUSH and h == 0:
                    fw.flush()
            fw.flush()

        with ExitStack() as ph:
            Mh = sb(ph, "Mh", [128, 2, 16, 256], BF16)
            wiq = sb(ph, "wiq", [128, 2, 512], BF16)
            wuvp = sb(ph, "wuvp", [128, 2, 16, 128], BF16)
            t_wt = T()
            F01 = [pst(ph, "F0", [128, 512], F32), pst(ph, "F1", [128, 512], F32)]
            FQ = pst(ph, "FQ", [128, 1024], F32)
            F45 = [pst(ph, "F4", [128, 512], F32), pst(ph, "F5", [128, 512], F32)]
            H0 = pst(ph, "AH0", [128, 8, 128], BF16)
            t_F01, t_FQ, t_F45 = Ts(2, True), Ts(2, True), Ts(2, True)
            t_H0 = T(True)
            with ExitStack() as ph2:
                wuq = sb(ph2, "wuq", [128, 2, 1024], BF16)
                wuqT = sb(ph2, "wuqT", [128, 8, 256], BF16)
                wuk = sb(ph2, "wuk", [128, 8, 256], BF16)
                for rc in range(2):
                    wload(wuq[:, rc, :], w_uq[rc * 128:(rc + 1) * 128, :], t_wt, 1024)
                    wload(wiq[:, rc, :], w_iq[rc * 128:(rc + 1) * 128, :], t_wt, 512)
                wukv = w_uk.rearrange("(hp two) d r -> (two d) hp r", two=2)
                for j in range(2):
                    wload3(wuk[:, j * 4:(j + 1) * 4, :], wukv[:, j * 4:(j + 1) * 4, :], t_wt, 4, 256)
                G("memset", [], [t_wt], ap=wuvp[:], constant=0.0)
                wuvv = w_uv.rearrange("h (rc r) d -> r rc h d", rc=2)
                for rc in range(2):
                    i = stg_i[0] % 3
                    stg_i[0] += 1
                    sv = stg[i][:, 0:1024].rearrange("p (h d) -> p h d", h=16)
                    fw.dma(sv, wuvv[:, rc, :, :], writes=[t_stg[i]])
                    for h in range(16):
                        G("tensor_copy", [t_stg[i]], [t_wt], out=wuvp[:, rc, h, (h % 2) * 64:(h % 2) * 64 + 64],
                          in_=sv[:, h, :])
                for rc in range(2):
                    for cb in range(8):
                        P("transpose", [t_wt, t_const], [t_H0], out=H0[:, cb, :],
                          in_=wuq[:, rc, cb * 128:(cb + 1) * 128], identity=ident_b[:])
                    V("tensor_copy", [t_H0], [t_wt], out=wuqT[:, :, rc * 128:(rc + 1) * 128], in_=H0[:])
                for h in range(16):
                    hp, two = h // 2, h % 2
                    ps_ = slice(two * 64, two * 64 + 64)
                    for rc in range(2):
                        mm(F01[rc][:, 0:256], wuqT[ps_, hp, rc * 128:(rc + 1) * 128], wuk[ps_, hp, :], True, True,
                           [t_wt], [t_F01[rc]])
                        A("copy", [t_F01[rc]], [t_wt], out=Mh[:, rc, h, :], in_=F01[rc][:, 0:256])
                fw.flush()

            sc = sb(ph, "sc", [128, L], F32)
            jnk = sb(ph, "jnk", [128, L], BF16)
            biasm = sb(ph, "biasm", [128, L], BF16)
            biasT = sb(ph, "biasT", [128, NT, 128], BF16)
            qlT = sb(ph, "qlT", [128, 2, 16, 128], BF16)
            qiT = sb(ph, "qiT", [64, 8, 128], BF16)
            rl = [sb(ph, "rl%d" % i, [128, 512], F32) for i in range(2)]
            pt = [sb(ph, "pt%d" % i, [128, 512], BF16) for i in range(2)]
            rden = sb(ph, "rden", [128, 512], F32)
            onT = sb(ph, "onT", [128, 2, 512], BF16)
            bis = sb(ph, "bis", [128, 8], F32)
            dl = sb(ph, "dl", [128, 32], F32)
            crow = sb(ph, "crow", [128, 32], F32)
            oaq = [sb(ph, "oaq%d" % i, [128, 8, 128], BF16) for i in range(2)]
            t_sc, t_jnk, t_biasm, t_biasT, t_qlT, t_qiT, t_rden, t_onT, t_bis = Ts(9)
            t_rl, t_pt, t_oaq = Ts(2), Ts(2), Ts(2)
            for k in range(32):
                V("memset", [], [t_bis], ap=crow[:, k:k + 1], constant=float(2.0 ** (-k)))

            AX = pst(ph, "AX", [128, 512], F32)
            t_AX = T(True)
            qlT2 = [qlT, sb(ph, "qlT_b", [128, 2, 16, 128], BF16)]
            biasT2 = [biasT, sb(ph, "biasT_b", [128, NT, 128], BF16)]
            t_qlT2, t_biasT2 = Ts(2), Ts(2)

            def stageA(qb):
                qs = slice(qb * 128, (qb + 1) * 128)
                Tk = (qb + 1) * 128
                qlT_, t_qlT_ = qlT2[qb % 2], t_qlT2[qb % 2]
                biasT_, t_biasT_ = biasT2[qb % 2], t_biasT2[qb % 2]
                for half in range(2):
                    for hh in range(4):
                        hi = half * 4 + hh
                        for rc in range(2):
                            mm(AX[0:64, hh * 128:(hh + 1) * 128], wiq[:, rc, hi * 64:(hi + 1) * 64], cqT[:, rc, qs],
                               rc == 0, rc == 1, [t_wt, t_cqT[qb]], [t_AX])
                    A("copy", [t_AX], [t_qiT], out=qiT[:, half * 4:(half + 1) * 4, :],
                      in_=AX[0:64, :].rearrange("p (a b) -> p a b", a=4))
                    yield
                for rco in range(2):
                    for hg in range(4):
                        for hl in range(4):
                            h = hg * 4 + hl
                            for rci in range(2):
                                mm(AX[:, hl * 128:(hl + 1) * 128], Mh[:, rci, h, rco * 128:(rco + 1) * 128],
                                   cqT[:, rci, qs], rci == 0, rci == 1, [t_wt, t_cqT[qb]], [t_AX])
                        yield
                        V("tensor_copy", [t_AX], [t_qlT_], out=qlT_[:, rco, hg * 4:(hg + 1) * 4, :],
                          in_=AX[:].rearrange("p (a b) -> p a b", a=4))
                for kg in range(qb // 4 + 1):
                    ncol = min(512, Tk - kg * 512)
                    ks = slice(kg * 512, kg * 512 + ncol)
                    kts = [t_kiT[j] for j in range(kg * 4, kg * 4 + ncol // 128)]
                    for hi in range(8):
                        b2 = hi % 2
                        mm(AX[:, 0:ncol], qiT[:, hi, :], kiT[:, ks], True, True, [t_qiT] + kts, [t_AX])
                        yield
                        A("activation", [t_AX, t_w], [t_rl[b2]], out=rl[b2][:, 0:ncol], in_=AX[:, 0:ncol],
                          func=AF.Relu, scale=absw[:, qb, hi:hi + 1])
                        if hi == 0:
                            V("tensor_scalar", [t_rl[b2], t_w], [t_sc], out=sc[:, ks], in0=rl[b2][:, 0:ncol],
                              scalar1=sgnw[:, qb, hi:hi + 1], scalar2=None, op0=ALU.mult)
                        else:
                            V("scalar_tensor_tensor", [t_rl[b2], t_w, t_sc], [t_sc], out=sc[:, ks],
                              in0=rl[b2][:, 0:ncol], scalar=sgnw[:, qb, hi:hi + 1], in1=sc[:, ks], op0=ALU.mult,
                              op1=ALU.add)
                if qb >= 2:
                    V("tensor_reduce", [t_sc], [t_bis], out=bis[:, 0:1], in_=sc[:, 0:Tk], axis=AX_.X, op=ALU.max,
                      apply_absolute_value=True)
                    V("tensor_scalar", [t_bis], [t_bis], out=dl[:], in0=crow[:], scalar1=bis[:, 0:1], scalar2=None,
                      op0=ALU.mult)
                    V("memset", [], [t_bis], ap=bis[:, 1:2], constant=0.0)
                G("affine_select", [t_sc], [t_sc], out=sc[:, qs], in_=sc[:, qs], pattern=[[-1, 128]],
                  compare_op=ALU.is_ge, fill=-1e30, base=0, channel_multiplier=1)
                yield
                if qb >= 2:
                    cur = 1
                    for k in range(NBIS):
                        nxt = 3 - cur
                        V("tensor_scalar", [t_sc, t_bis], [t_jnk, t_bis], out=jnk[:, 0:Tk], in0=sc[:, 0:Tk],
                          scalar1=bis[:, cur:cur + 1], scalar2=0.0, op0=ALU.is_ge, op1=ALU.add,
                          accum_out=bis[:, 3:4])
                        V("tensor_scalar", [t_bis], [t_bis], out=bis[:, 4:5], in0=bis[:, 3:4], scalar1=255.5,
                          scalar2=0.5, op0=ALU.is_gt, op1=ALU.subtract)
                        V("scalar_tensor_tensor", [t_bis], [t_bis], out=bis[:, nxt:nxt + 1], in0=bis[:, 4:5],
                          scalar=dl[:, k:k + 1], in1=bis[:, cur:cur + 1], op0=ALU.mult, op1=ALU.add)
                        cur = nxt
                        yield
                    V("tensor_tensor", [t_bis], [t_bis], out=bis[:, 5:6], in0=bis[:, cur:cur + 1],
                      in1=dl[:, NBIS:NBIS + 1], op=ALU.subtract)
                else:
                    V("memset", [], [t_bis], ap=bis[:, 5:6], constant=-1e29)
                V("tensor_scalar", [t_sc, t_bis], [t_biasm], out=biasm[:, 0:Tk], in0=sc[:, 0:Tk], scalar1=bis[:, 5:6],
                  scalar2=NEG, op0=ALU.is_lt, op1=ALU.mult)
                yield
                for kb0 in range(0, qb + 1, 8):
                    nk = min(8, qb + 1 - kb0)
                    for j in range(nk):
                        kb = kb0 + j
                        P("transpose", [t_biasm, t_const], [t_H0], out=H0[:, j, :],
                          in_=biasm[:, kb * 128:(kb + 1) * 128], identity=ident_b[:])
                    A("copy", [t_H0], [t_biasT_], out=biasT_[:, kb0:kb0 + nk, :], in_=H0[:, 0:nk, :])
                    yield

            def stageE(qb, filler):
                qs = slice(qb * 128, (qb + 1) * 128)
                qlT_, t_qlT_ = qlT2[qb % 2], t_qlT2[qb % 2]
                biasT_, t_biasT_ = biasT2[qb % 2], t_biasT2[qb % 2]
                psO = [FQ[:, 0:512], FQ[:, 512:1024]]
                psD = F45[0]
                psOA = F45[1]
                oq = oaq[qb % 2]
                t_oq = t_oaq[qb % 2]

                def fill(n):
                    for _ in range(n):
                        if filler is not None:
                            next(filler, None)

                def qk_scores(hg, kb):
                    b2 = kb % 2
                    kbs = slice(kb * 128, (kb + 1) * 128)
                    for rc in range(2):
                        mm(F01[b2][:], ckvT[:, rc, kbs],
                           qlT_[:, rc, hg * 4:(hg + 1) * 4, :].rearrange("p a b -> p (a b)"),
                           rc == 0, False, [t_ckvT[kb], t_qlT_], [t_F01[b2]])
                    mm(F01[b2][:], ident_b[:], biasT_[:, kb:kb + 1, :].to_broadcast([128, 4, 128]), False, True,
                       [t_const, t_biasT_], [t_F01[b2]])

                for hg in range(4):
                    qk_scores(hg, 0)
                    for kb in range(qb + 1):
                        b2 = kb % 2
                        if kb < qb:
                            qk_scores(hg, kb + 1)
                        A("activation", [t_F01[b2]], [t_pt[b2]], out=pt[b2][:], in_=F01[b2][:], func=AF.Exp,
                          scale=0.125)
                        for rc in range(2):
                            mm(psO[rc], ckv1[:, kb, rc * 128:(rc + 1) * 128], pt[b2][:], kb == 0, kb == qb,
                               [t_ckv1[kb], t_pt[b2]], [t_FQ[rc]])
                        mm(psD[:], ones_b[:], pt[b2][:], kb == 0, kb == qb, [t_const, t_pt[b2]], [t_F45[0]])
                        fill(1)
                    V("reciprocal", [t_F45[0]], [t_rden], out=rden[:], in_=psD[:])
                    for rc in range(2):
                        V("tensor_tensor", [t_FQ[rc], t_rden], [t_onT], out=onT[:, rc, :], in0=psO[rc], in1=rden[:],
                          op=ALU.mult)
                    for hpl in range(2):
                        hp = hg * 2 + hpl
                        k = 0
                        for two in range(2):
                            h = hp * 2 + two
                            hl = h - hg * 4
                            for rc in range(2):
                                mm(psOA[:, hpl * 128:(hpl + 1) * 128], wuvp[:, rc, h, :],
                                   onT[:, rc, hl * 128:(hl + 1) * 128], k == 0, k == 3, [t_wt, t_onT], [t_F45[1]])
                                k += 1
                    A("copy", [t_F45[1]], [t_oq], out=oq[:, hg * 2:hg * 2 + 2, :],
                      in_=psOA[:, 0:256].rearrange("p (a b) -> p a b", a=2))
                fw.dma(oa_v[:, :, qs], oq[:], reads=[t_oq], writes=[t_oad])

            for _ in stageA(0):
                pass
            for qb in range(NT):
                gen = stageA(qb + 1) if qb + 1 < NT else None
                stageE(qb, gen)
                if gen is not None:
                    for _ in gen:
                        pass
            fw.flush()
        att.close()

        with ExitStack() as ph:
            PA = sb(ph, "PA", [128, 8, D], BF16)
            PB = sb(ph, "PB", [128, 8, D], BF16)
            WO = sb(ph, "WO", [128, 8, D], BF16)
            t_PA, t_PB, t_WO = Ts(3)
            wga = sb(ph, "wga", [128, 8, D], BF16)
            wgb = sb(ph, "wgb", [128, 8, D], BF16)
            t_wga, t_wgb = T(), T()
            oab = sb(ph, "oab", [128, 8, 512], BF16)
            obb = sb(ph, "obb", [128, 8, 512], BF16)
            t_oab, t_obb = T(), T()
            sgA = sb(ph, "sgA", [128, 512], F32)
            sgB = sb(ph, "sgB", [128, 512], F32)
            tA = sb(ph, "tA", [128, 512], F32)
            tB = sb(ph, "tB", [128, 512], F32)
            t_sgA, t_sgB, t_tA, t_tB = Ts(4)
            mT = sb(ph, "mT", [128, 8, 512], BF16)
            t_mT = T()
            xt2 = [sb(ph, "xt2_%d" % i, [128, D], F32) for i in range(2)]
            x2t = [sb(ph, "x2t_%d" % i, [128, D], F32) for i in range(2)]
            t_xt2, t_x2t = Ts(2), Ts(2)
            B = [pst(ph, "MB%d" % i, [128, 512], F32) for i in range(6)]
            t_B = Ts(6, True)
            cast_engs[:] = ["scalar", "vector"]
            for kc in range(8):
                ks_ = slice(kc * 128, (kc + 1) * 128)
                wload(PA[:, kc, :], w_ba[ks_, :], t_PA, 1024)
                wload(PB[:, kc, :], w_bb[ks_, :], t_PB, 1024)
                wload(WO[:, kc, :], w_out[ks_, :], t_WO, 1024)
                wload(wga[:, kc, :], w_in_v[:, kc, C_GA:C_GA + D], t_wga, 1024)
                wload(wgb[:, kc, :], w_in_v[:, kc, C_GB:C_GB + D], t_wgb, 1024)
            cnt = 0
            for tb in range(4):
                tsl = slice(tb * 512, (tb + 1) * 512)
                fw.dma(oab[:], oa_v[:, :, tsl], reads=[t_oad], writes=[t_oab])
                fw.dma(obb[:], ob_v[:, :, tsl], reads=[t_obd], writes=[t_obb])
                for nc_ in range(8):
                    i2 = cnt % 2
                    cnt += 1
                    ncs = slice(nc_ * 128, (nc_ + 1) * 128)
                    hts = t_hT[tb * 4:(tb + 1) * 4]
                    for kc in range(8):
                        mm(B[0][:], wga[:, kc, ncs], hT[:, kc, tsl], kc == 0, kc == 7, [t_wga] + hts, [t_B[0]])
                    for kc in range(8):
                        mm(B[1][:], wgb[:, kc, ncs], hT[:, kc, tsl], kc == 0, kc == 7, [t_wgb] + hts, [t_B[1]])
                    A("activation", [t_B[0]], [t_sgA], out=sgA[:], in_=B[0][:], func=AF.Sigmoid)
                    A("activation", [t_B[1]], [t_sgB], out=sgB[:], in_=B[1][:], func=AF.Sigmoid)
                    for hp in range(8):
                        mm(B[2][:], PA[:, hp, ncs], oab[:, hp, :], hp == 0, hp == 7, [t_PA, t_oab], [t_B[2]])
                    for hh in range(8):
                        mm(B[3][:], PB[:, hh, ncs], obb[:, hh, :], hh == 0, hh == 7, [t_PB, t_obb], [t_B[3]])
                    V("tensor_tensor", [t_B[2], t_sgA], [t_tA], out=tA[:], in0=B[2][:], in1=sgA[:], op=ALU.mult)
                    V("tensor_tensor", [t_B[3], t_sgB], [t_tB], out=tB[:], in0=B[3][:], in1=sgB[:], op=ALU.mult)
                    G("tensor_tensor", [t_tA, t_tB], [t_mT], out=mT[:, nc_, :], in0=tA[:], in1=tB[:], op=ALU.add)
                for j in range(4):
                    tt = tb * 4 + j
                    i2 = tt % 2
                    rows = slice(tt * 128, (tt + 1) * 128)
                    fw.dma(xt2[i2][:], x_d[rows, :], writes=[t_xt2[i2]])
                    for half in range(2):
                        hs = slice(half * 512, (half + 1) * 512)
                        for nc_ in range(8):
                            mm(B[4 + half][:], mT[:, nc_, j * 128:(j + 1) * 128], WO[:, nc_, hs], nc_ == 0, nc_ == 7,
                               [t_mT, t_WO], [t_B[4 + half]])
                        V("tensor_tensor", [t_B[4 + half], t_xt2[i2]], [t_x2t[i2]], out=x2t[i2][:, hs],
                          in0=B[4 + half][:], in1=xt2[i2][:, hs], op=ALU.add)
                    fw.dma(x2_d[rows, :], x2t[i2][:], reads=[t_x2t[i2]], writes=[t_x2d])
            fw.flush()
        mix.close()

        with ExitStack() as ph:
            Wg = sb(ph, "Wg", [128, 8, DFF], BF16)
            Wu = sb(ph, "Wu", [128, 8, DFF], BF16)
            Wd = sb(ph, "Wd", [128, NFC, D], BF16)
            t_Wg, t_Wu, t_Wd = Ts(3)
            gBf = sb(ph, "gBf", [128, 8, 128], F32)
            ffng = sb(ph, "ffng", [128, 8], F32)
            fing = sb(ph, "fing", [128, D], F32)
            t_fg = T()
            x2t = [sb(ph, "fx2t_%d" % i, [128, D], F32) for i in range(2)]
            t_x2t = Ts(2)
            x3 = sb(ph, "x3", [128, D], F32)
            yo = sb(ph, "yo", [128, D], F32)
            xs = sb(ph, "fxs", [128, D], BF16)
            ss = sb(ph, "fss", [128, 8], F32)
            fs = sb(ph, "ffs", [128, 8], F32)
            h2T = sb(ph, "h2T", [128, 8, 256], BF16)
            act = sb(ph, "act", [128, NFC, 256], BF16)
            sg = [sb(ph, "sg%d" % i, [128, 256], F32) for i in range(2)]
            t_x3, t_yo, t_fs, t_h2T, t_act = Ts(5)
            t_sg = Ts(2)
            pT = pst(ph, "fpT", [128, 8, 128], BF16)
            B = [pst(ph, "FB%d" % i, [128, 512], F32) for i in range(6)]
            t_B = Ts(6, True)
            res = (yo, t_yo, ss, T(), xs, T(), pT, T(True))
            fw.dma(ffng[:], ffng_d, writes=[t_fg])
            fw.dma(fing[:], fing_d, writes=[t_fg])
            for kc in range(8):
                V("tensor_scalar", [t_const, t_fg], [t_fg], out=gBf[:, kc, :], in0=ones_f[:],
                  scalar1=ffng[:, kc:kc + 1], scalar2=None, op0=ALU.mult)
            for kc in range(8):
                ks_ = slice(kc * 128, (kc + 1) * 128)
                for c0 in (0, 1024, 2048):
                    w_ = min(1024, DFF - c0)
                    wload(Wg[:, kc, c0:c0 + w_], w_gate[ks_, c0:c0 + w_], t_Wg, w_)
                    wload(Wu[:, kc, c0:c0 + w_], w_up[ks_, c0:c0 + w_], t_Wu, w_)
            for fc in range(NFC):
                wload(Wd[:, fc, :], w_down[fc * 128:(fc + 1) * 128, :], t_Wd, 1024)
            for tb2 in range(8):
                for j in range(2):
                    tt = tb2 * 2 + j
                    fw.dma(x2t[j][:], x2_d[tt * 128:(tt + 1) * 128, :], reads=[t_x2d], writes=[t_x2t[j]])
                    rmsnorm_T("ffn", x2t[j][:], t_x2t[j], gBf, h2T, t_h2T, j * 128, res)
                for fc in range(NFC):
                    bk = fc % 2
                    fcs = slice(fc * 128, (fc + 1) * 128)
                    for kc in range(8):
                        mm(B[bk][:, 0:256], Wg[:, kc, fcs], h2T[:, kc, :], kc == 0, kc == 7, [t_Wg, t_h2T], [t_B[bk]])
                    for kc in range(8):
                        mm(B[bk][:, 256:512], Wu[:, kc, fcs], h2T[:, kc, :], kc == 0, kc == 7, [t_Wu, t_h2T],
                           [t_B[bk]])
                    A("activation", [t_B[bk]], [t_sg[bk]], out=sg[bk][:], in_=B[bk][:, 0:256], func=AF.Silu)
                    V("tensor_tensor", [t_B[bk], t_sg[bk]], [t_act], out=act[:, fc, :], in0=B[bk][:, 256:512],
                      in1=sg[bk][:], op=ALU.mult)
                for j in range(2):
                    tt = tb2 * 2 + j
                    for half in range(2):
                        bi = 2 + j * 2 + half
                        hs = slice(half * 512, (half + 1) * 512)
                        for fc in range(NFC):
                            mm(B[bi][:], act[:, fc, j * 128:(j + 1) * 128], Wd[:, fc, hs], fc == 0, fc == NFC - 1,
                               [t_act, t_Wd], [t_B[bi]])
                        V("tensor_tensor", [t_B[bi], t_x2t[j]], [t_x3], out=x3[:, hs], in0=B[bi][:],
                          in1=x2t[j][:, hs], op=ALU.add)
                    A("activation", [t_x3], [t_yo, t_fs], out=yo[:], in_=x3[:], func=AF.Square, accum_out=fs[:, 0:1])
                    rstd(t_fs, fs[:, 0:1], fs[:, 1:2], fs[:, 3:4], 1.0 / D, EPS)
                    V("scalar_tensor_tensor", [t_x3, t_fs, t_fg], [t_yo], out=yo[:], in0=x3[:], scalar=fs[:, 3:4],
                      in1=fing[:], op0=ALU.mult, op1=ALU.mult)
                    fw.dma(out_d[tt * 128:(tt + 1) * 128, :], yo[:], reads=[t_yo])
            fw.flush()
        print("bass ops recorded:", fw.nops)
    except _Stop:
        print("build truncated after flush", STOP_AFTER_FLUSH)
    return nc


_NC_CACHE = {}


def _rep(v, n=128):
    return np.ascontiguousarray(np.tile(np.asarray(v, np.float32).reshape(1, -1), (n, 1)))


def kernel(x, mix_norm_g, w_in, cq_norm_g, ckv_norm_g, w_uq, w_uk, w_uv, w_iq,
           kidx_ln_g, kidx_ln_b, w_branch_a, conv_w, a_log, dt_bias, onorm_g,
           w_branch_b, w_out, ffn_norm_g, w_gate, w_up, w_down, final_norm_g):
    f = lambda a: np.ascontiguousarray(np.asarray(a, dtype=np.float32))
    x = f(x)
    n = x.shape[0]
    if "nc" not in _NC_CACHE:
        _NC_CACHE["nc"] = build_nc()
    nc = _NC_CACHE["nc"]
    shared = {
        "w_in": f(w_in)[0], "w_uq": f(w_uq)[0], "w_uk": f(w_uk)[0], "w_uv": f(w_uv)[0], "w_iq": f(w_iq)[0],
        "w_branch_a": f(w_branch_a)[0], "w_branch_b": f(w_branch_b)[0], "w_out": f(w_out)[0],
        "w_gate": f(w_gate)[0], "w_up": f(w_up)[0], "w_down": f(w_down)[0],
        "mixg_col": np.ascontiguousarray(f(mix_norm_g)[0].reshape(8, 128).T),
        "ffng_col": np.ascontiguousarray(f(ffn_norm_g)[0].reshape(8, 128).T),
        "fing_row": _rep(final_norm_g),
        "cqg_row": _rep(f(cq_norm_g)[0]), "ckvg_row": _rep(f(ckv_norm_g)[0]),
        "klng_row": _rep(f(kidx_ln_g)[0]), "klnb_row": _rep(f(kidx_ln_b)[0]),
        "conv_col": np.ascontiguousarray(f(conv_w)[0].T.reshape(24, 128, 4).transpose(1, 0, 2)),
        "alog_row": _rep(f(a_log)[0]), "dtb_row": _rep(f(dt_bias)[0]),
        "onormg_col": np.ascontiguousarray(f(onorm_g)[0].reshape(128, 1)),
        "ident": np.eye(128, dtype=np.float32),
        "uincl": np.triu(np.ones((128, 128), np.float32)),
        "lstrict": np.tril(np.ones((128, 128), np.float32), -1),
    }
    in_maps = [dict(shared, x=x[i]) for i in range(n)]
    res = run_bass_kernel_spmd(nc, in_maps, core_ids=list(range(n)))
    out = np.stack([np.asarray(r["out"], dtype=np.float32) for r in res.results], axis=0)
    if DEBUG:
        kernel.debug = res.results
    return out
```
